# Optimizing a Trainium2 kernel written in Bass

```python
import math
import jax, jax.numpy as jnp
from jax import lax
import numpy as np

D_MODEL = 1024
BATCH = 8
SEQ = 4096
DEPTH = 1

POOL_WINDOWS = (2, 4, 8, 16)
POOL_GROUPS = 4
POOL_WIDTH = D_MODEL // 2
POOL_GROUP_DIM = POOL_WIDTH // POOL_GROUPS
N_HEADS = 8
HEAD_DIM = 64
ATTN_WIDTH = N_HEADS * HEAD_DIM
KV_DIM = HEAD_DIM
IDX_HEADS = 8
IDX_DIM = 32
TOPK_MAX = 256
Q_BLOCK = 128
NEG_INF = -1e30
N_BRANCHES = 2
N_GROUPS = 4
EXPERTS_PER_GROUP = 8
N_EXPERTS = N_GROUPS * EXPERTS_PER_GROUP
EXPERT_FF = 256
ROUTE_TOPK = 2
LN_EPS = 1e-5
DEEPNORM_ALPHA = (2.0 * DEPTH) ** 0.25
DEEPNORM_BETA = (8.0 * DEPTH) ** -0.25
SPLITS = (POOL_WIDTH, ATTN_WIDTH, KV_DIM, KV_DIM, IDX_HEADS * IDX_DIM, IDX_DIM, IDX_HEADS, N_BRANCHES * D_MODEL)
IN_WIDTH = 3496

kernel_name = 'hybrid_pool_dsa_hmoe'


def layer_norm(x, g, b):
    x32 = x.astype(jnp.float32)
    mu = jnp.mean(x32, axis=-1, keepdims=True)
    xc = x32 - mu
    var = jnp.mean(xc * xc, axis=-1, keepdims=True)
    y = xc * lax.rsqrt(var + LN_EPS) * g.astype(jnp.float32) + b.astype(jnp.float32)
    return y.astype(x.dtype)


def causal_multiscale_pool(u, w_group, scale):
    B, L, _ = u.shape
    u32 = u.astype(jnp.float32).reshape(B, L, POOL_GROUPS, POOL_GROUP_DIM)
    csum = jnp.concatenate([jnp.zeros_like(u32[:, :1]), jnp.cumsum(u32, axis=1)], axis=1)
    pos = jnp.arange(L)
    deltas = []
    for g, w in enumerate(POOL_WINDOWS):
        start = jnp.maximum(pos + 1 - w, 0)
        win_sum = csum[:, 1:, g] - csum[:, start, g]
        count = (pos + 1 - start).astype(jnp.float32)
        deltas.append(win_sum / count[None, :, None] - u32[:, :, g])
    delta = jnp.stack(deltas, axis=2)
    mixed = jnp.einsum('blgd,gde->blge', delta, w_group.astype(jnp.float32))
    out = mixed.reshape(B, L, POOL_WIDTH) * scale.astype(jnp.float32)
    return out.astype(u.dtype)


def dsa_attention(q, k, v, iq, ik, iw):
    B, L, H, dh = q.shape
    n_sel = min(TOPK_MAX, L // 4)
    nb = L // Q_BLOCK
    sm_scale = 1.0 / math.sqrt(dh)
    key_pos = jnp.arange(L)
    ik32 = ik.astype(jnp.float32)

    def to_blocks(a):
        return a.reshape((B, nb, Q_BLOCK) + a.shape[2:]).swapaxes(0, 1)

    q_b, iq_b, iw_b = to_blocks(q), to_blocks(iq), to_blocks(iw)
    qpos_b = key_pos.reshape(nb, Q_BLOCK)

    def block(args):
        qb, iqb, iwb, qpos = args
        idx_logits = jnp.einsum('bqhd,bsd->bqhs', iqb.astype(jnp.float32), ik32)
        score = jnp.einsum('bqh,bqhs->bqs', iwb.astype(jnp.float32), jax.nn.relu(idx_logits))
        admissible = key_pos[None, :] <= qpos[:, None]
        score = jnp.where(admissible[None], score, NEG_INF)
        _, sel = lax.top_k(score, n_sel)
        valid = sel <= qpos[None, :, None]
        k_sel = jax.vmap(lambda kk, ii: kk[ii])(k, sel)
        v_sel = jax.vmap(lambda vv, ii: vv[ii])(v, sel)
        logits = jnp.einsum('bqhd,bqkd->bqhk', qb.astype(jnp.float32), k_sel.astype(jnp.float32)) * sm_scale
        logits = jnp.where(valid[:, :, None, :], logits, NEG_INF)
        p = jax.nn.softmax(logits, axis=-1)
        o = jnp.einsum('bqhk,bqkd->bqhd', p, v_sel.astype(jnp.float32))
        return o.astype(q.dtype)

    out = lax.map(block, (q_b, iq_b, iw_b, qpos_b))
    return out.swapaxes(0, 1).reshape(B, L, H * dh)


def hierarchical_moe(h, w_group, b_group, w_router, b_router, w_gate, w_up, w_down):
    B, L, D = h.shape
    t = h.reshape(B * L, D)
    t32 = t.astype(jnp.float32)
    group_logits = t32 @ w_group.astype(jnp.float32) + b_group.astype(jnp.float32)
    group_prob = jax.nn.softmax(group_logits, axis=-1)
    g_sel = jnp.argmax(group_logits, axis=-1)
    p_group = jnp.take_along_axis(group_prob, g_sel[:, None], axis=-1)
    exp_logits = (t32 @ w_router.astype(jnp.float32) + b_router.astype(jnp.float32)).reshape(-1, N_GROUPS, EXPERTS_PER_GROUP)
    exp_logits = jnp.take_along_axis(exp_logits, g_sel[:, None, None], axis=1)[:, 0]
    top_vals, top_idx = lax.top_k(exp_logits, ROUTE_TOPK)
    top_w = jax.nn.softmax(top_vals, axis=-1) * p_group
    expert_id = g_sel[:, None] * EXPERTS_PER_GROUP + top_idx
    gates = jnp.sum(jax.nn.one_hot(expert_id, N_EXPERTS, dtype=jnp.float32) * top_w[..., None], axis=1)
    out = jnp.zeros((B * L, D), jnp.float32)
    for e in range(N_EXPERTS):
        hid = jax.nn.silu(t @ w_gate[e]) * (t @ w_up[e])
        out = out + gates[:, e:e + 1] * (hid @ w_down[e]).astype(jnp.float32)
    return out.reshape(B, L, D).astype(h.dtype)


def setup_inputs(seed: int = 0) -> dict:
    key = jax.random.key(seed)
    ks = jax.random.split(key, 24)
    f32 = jnp.float32
    D = D_MODEL

    def nrm(k, shape, scale):
        return jax.random.normal(k, shape, f32) * scale

    col_scale = jnp.concatenate([
        jnp.ones((POOL_WIDTH + ATTN_WIDTH + KV_DIM,), f32),
        jnp.full((KV_DIM,), DEEPNORM_BETA, f32),
        jnp.ones((IDX_HEADS * IDX_DIM + IDX_DIM + IDX_HEADS + N_BRANCHES * D,), f32)])
    return {
        'x': jax.random.normal(ks[0], (BATCH, SEQ, D), f32),
        'ln_in_g': 1.0 + nrm(ks[1], (D,), 0.01),
        'ln_in_b': nrm(ks[2], (D,), 0.01),
        'w_in': nrm(ks[3], (DEPTH, D, IN_WIDTH), D ** -0.5) * col_scale,
        'b_gate': nrm(ks[4], (DEPTH, N_BRANCHES * D), 0.1),
        'pool_w': nrm(ks[5], (DEPTH, POOL_GROUPS, POOL_GROUP_DIM, POOL_GROUP_DIM), POOL_GROUP_DIM ** -0.5),
        'pool_scale': 1.0 + nrm(ks[6], (DEPTH, POOL_WIDTH), 0.01),
        'w_proj_pool': nrm(ks[7], (DEPTH, POOL_WIDTH, D), POOL_WIDTH ** -0.5),
        'w_proj_attn': nrm(ks[8], (DEPTH, ATTN_WIDTH, D), ATTN_WIDTH ** -0.5),
        'w_out': nrm(ks[9], (DEPTH, D, D), D ** -0.5 * DEEPNORM_BETA),
        'ln1_g': 1.0 + nrm(ks[10], (DEPTH, D), 0.01),
        'ln1_b': nrm(ks[11], (DEPTH, D), 0.01),
        'w_group': nrm(ks[12], (DEPTH, D, N_GROUPS), D ** -0.5),
        'b_group': nrm(ks[13], (DEPTH, N_GROUPS), 0.01),
        'w_router': nrm(ks[14], (DEPTH, D, N_EXPERTS), D ** -0.5),
        'b_router': nrm(ks[15], (DEPTH, N_EXPERTS), 0.01),
        'w_gate': nrm(ks[16], (DEPTH, N_EXPERTS, D, EXPERT_FF), D ** -0.5),
        'w_up': nrm(ks[17], (DEPTH, N_EXPERTS, D, EXPERT_FF), D ** -0.5),
        'w_down': nrm(ks[18], (DEPTH, N_EXPERTS, EXPERT_FF, D), EXPERT_FF ** -0.5 * DEEPNORM_BETA),
        'ln2_g': 1.0 + nrm(ks[19], (DEPTH, D), 0.01),
        'ln2_b': nrm(ks[20], (DEPTH, D), 0.01),
    }


def reference(x, ln_in_g, ln_in_b, w_in, b_gate, pool_w, pool_scale, w_proj_pool, w_proj_attn, w_out,
              ln1_g, ln1_b, w_group, b_group, w_router, b_router, w_gate, w_up, w_down, ln2_g, ln2_b):
    B, L, D = x.shape
    offsets = [int(o) for o in np.cumsum(SPLITS)[:-1]]
    h = layer_norm(x, ln_in_g, ln_in_b)
    for l in range(DEPTH):
        proj = h @ w_in[l]
        u_pool, q, k, v, iq, ik, iw, gate_pre = jnp.split(proj, offsets, axis=-1)
        pool_out = causal_multiscale_pool(u_pool, pool_w[l], pool_scale[l])
        attn_out = dsa_attention(q.reshape(B, L, N_HEADS, HEAD_DIM), k, v,
                                 iq.reshape(B, L, IDX_HEADS, IDX_DIM), ik, iw)
        gates = jax.nn.sigmoid(gate_pre.astype(jnp.float32) + b_gate[l].astype(jnp.float32)).reshape(B, L, N_BRANCHES, D)
        branch_pool = (pool_out @ w_proj_pool[l]).astype(jnp.float32)
        branch_attn = (attn_out @ w_proj_attn[l]).astype(jnp.float32)
        merged = gates[:, :, 0] * branch_pool + gates[:, :, 1] * branch_attn
        mix = merged.astype(h.dtype) @ w_out[l]
        h = layer_norm(DEEPNORM_ALPHA * h + mix, ln1_g[l], ln1_b[l])
        ffn = hierarchical_moe(h, w_group[l], b_group[l], w_router[l], b_router[l], w_gate[l], w_up[l], w_down[l])
        h = layer_norm(DEEPNORM_ALPHA * h + ffn, ln2_g[l], ln2_b[l])
    return h.astype(x.dtype)
```

```python
import numpy as np
import ml_dtypes
from contextlib import ExitStack
import concourse.bass as bass
import concourse.mybir as mybir
from concourse.bass_utils import run_bass_kernel_spmd

F32 = mybir.dt.float32
BF16 = mybir.dt.bfloat16
AF = mybir.ActivationFunctionType
ALU = mybir.AluOpType
AX = mybir.AxisListType

T = 4096
NT = 32
D = 1024
KC = 8
ALPHA = 2.0 ** 0.25
EPS = 1e-5
SM_SCALE = 0.125
NSEL = 256
NBIS = 20
CTOK = 512 + 2048 + 64 + 8
CFEAT = 512 + 64 + 256 + 96
NEG = -30000.0
ACT_SPLIT_MIN = 1536


class Tok:
    __slots__ = ("sem", "val", "need", "dma")


class Buf:
    def __init__(self):
        self.w = {}
        self.r = {}


class Prog:
    def __init__(self, nc, es):
        self.nc = nc
        self.es = es
        self.L = {e: [] for e in ("pe", "act", "dve", "pool", "sp")}
        self.esem = {e: es.enter_context(nc.semaphore("s_" + e)) for e in self.L}
        self.dsem = {}
        self.dcnt = {}
        self.nops = 0

    def _deps(self, reads, writes, extra):
        deps = {}
        for b in reads:
            for k, t in b.w.items():
                deps[(k, t.val if t.dma else id(t))] = t
        for b in writes:
            for k, t in b.w.items():
                deps[(k, t.val if t.dma else id(t))] = t
            for k, t in b.r.items():
                deps[(k, t.val if t.dma else id(t))] = t
        for t in extra:
            if t is not None:
                deps[(id(t.sem), id(t))] = t
        return list(deps.values())

    def _post(self, t, reads, writes, acc):
        k = id(t.sem)
        for b in reads:
            b.r[k] = t
        for b in writes:
            if not acc:
                b.w = {}
            b.r = {}
            b.w[k] = t

    def op(self, eng, fn, reads=(), writes=(), acc=False, extra=()):
        L = self.L[eng]
        for d in self._deps(reads, writes, extra):
            if eng == "pe" and d.sem is self.esem["pe"]:
                continue
            d.need = True
            L.append(d)
        t = Tok()
        t.sem = self.esem[eng]
        t.val = None
        t.need = False
        t.dma = False
        L.append((fn, t))
        self._post(t, reads, writes, acc)
        self.nops += 1
        return t

    def dma(self, eng, out, in_, reads=(), writes=(), acc=False, extra=(), stream="d"):
        if stream not in self.dsem:
            self.dsem[stream] = self.es.enter_context(self.nc.semaphore("d_" + stream))
            self.dcnt[stream] = 0
        L = self.L[eng]
        for d in self._deps(reads, writes, extra):
            d.need = True
            L.append(d)
        self.dcnt[stream] += 16
        t = Tok()
        t.sem = self.dsem[stream]
        t.val = self.dcnt[stream]
        t.need = True
        t.dma = True
        L.append((lambda e: e.dma_start(out=out, in_=in_), t))
        self._post(t, reads, writes, acc)
        return t

    def finalize(self, final_waits):
        for e, L in self.L.items():
            c = 0
            for it in L:
                if isinstance(it, tuple):
                    fn, t = it
                    if (not t.dma) and t.need:
                        c += 1
                        t.val = c
        nc = self.nc

        def replay(e, eng):
            waited = {}
            for it in self.L[e]:
                if isinstance(it, Tok):
                    k = id(it.sem)
                    if waited.get(k, 0) >= it.val:
                        continue
                    eng.wait_ge(it.sem, it.val)
                    waited[k] = it.val
                else:
                    fn, t = it
                    ins = fn(eng)
                    if t.dma:
                        ins.then_inc(t.sem, 16)
                    elif t.need:
                        ins.then_inc(t.sem, 1)
            if e == "sp":
                for t in final_waits:
                    eng.wait_ge(t.sem, t.val)

        with nc.Block() as block:
            @block.tensor
            def _(eng):
                replay("pe", eng)

            @block.scalar
            def _(eng):
                replay("act", eng)

            @block.vector
            def _(eng):
                replay("dve", eng)

            @block.gpsimd
            def _(eng):
                replay("pool", eng)

            @block.sync
            def _(eng):
                replay("sp", eng)


class PsumPool:
    def __init__(self, banks):
        self.banks = banks
        self.i = 0

    def get(self):
        b = self.banks[self.i % len(self.banks)]
        self.i += 1
        return b


def build(cfg):
    ntiles = cfg.get("ntiles", NT)
    dbg = cfg.get("dbg", ())
    do_moe = cfg.get("moe", True)
    nexp = cfg.get("nexp", 32)
    nc = bass.Bass("TRN2", target_bir_lowering=False)
    es = ExitStack()
    with es:
        P = Prog(nc, es)

        def dram_in(name, shape, dt=F32):
            return nc.dram_tensor(name, list(shape), dt, kind="ExternalInput").ap()

        def dram_out(name, shape, dt=F32):
            return nc.dram_tensor(name, list(shape), dt, kind="ExternalOutput").ap()

        def sb(name, shape, dt):
            return es.enter_context(nc.sbuf_tensor("sb_" + name, list(shape), dt))

        B_ = {}

        def bufof(name):
            if name not in B_:
                B_[name] = Buf()
            return B_[name]

        x_d = dram_in("x", [T, D])
        wtok_d = dram_in("w_in_tok", [D, CTOK]).rearrange("(kc p) c -> p kc c", p=128)
        wfeat_d = dram_in("w_in_feat", [D, CFEAT]).rearrange("(kc p) c -> p kc c", p=128)
        lng_d = dram_in("ln_in_g", [128, D])
        lnb_d = dram_in("ln_in_b", [128, D])
        ln1g_d = dram_in("ln1_g", [128, D])
        ln1b_d = dram_in("ln1_b", [128, D])
        ln2g_d = dram_in("ln2_g", [128, D])
        ln2b_d = dram_in("ln2_b", [128, D])
        lngc_d = dram_in("ln_in_gc", [128, KC])
        lnbc_d = dram_in("ln_in_bc", [128, KC])
        ln1gc_d = dram_in("ln1_gc", [128, KC])
        ln1bc_d = dram_in("ln1_bc", [128, KC])
        ident_d = dram_in("ident", [128, 128], BF16)
        i4_d = dram_in("i4", [128, 512], BF16)
        band_d = dram_in("band", [128, 12 * 128], BF16)
        cbias_d = dram_in("cbias", [128, 128])
        pow2_d = dram_in("pow2", [128, NBIS + 1])
        bgate_d = dram_in("b_gate", [1, 2048])
        poolw_d = dram_in("pool_w", [4, 128, 128]).rearrange("g d e -> d g e")
        pscale_d = dram_in("pool_scale", [128, 4])
        wpp_d = dram_in("w_proj_pool", [512, D]).rearrange("(g p) c -> p g c", p=128)
        wpa_d = dram_in("w_proj_attn", [512, D]).rearrange("(g p) c -> p g c", p=128)
        wout_d = dram_in("w_out", [D, D]).rearrange("(g p) c -> p g c", p=128)
        wr_d = dram_in("w_gr", [D, 36]).rearrange("(g p) c -> p g c", p=128)
        br_d = dram_in("b_gr", [128, 36])
        wg_d = dram_in("w_gate", [32, D, 256]).rearrange("e (kc p) f -> e p kc f", p=128)
        wu_d = dram_in("w_up", [32, D, 256]).rearrange("e (kc p) f -> e p kc f", p=128)
        wd_d = dram_in("w_down", [32, 256, D]).rearrange("e (fc p) d -> e p fc d", p=128)
        out_d = dram_out("out", [T, D])
        h2_d = nc.dram_tensor("h2_scr", [T, D], F32, kind="Internal").ap()
        h2T_d = nc.dram_tensor("h2T_scr", [128, KC, T], BF16, kind="Internal").ap()
        hscr_d = nc.dram_tensor("h_scr", [T, D], F32, kind="Internal").ap()
        dbg_d = {}
        for name, shape in dbg:
            dbg_d[name] = dram_out("dbg_" + name, shape)

        A_WTOK, A_WFEAT, A_WOUT = 0, KC * CTOK, KC * CTOK + KC * CFEAT
        A_WPP = A_WOUT + KC * D
        A_WPA = A_WPP + 4 * D
        A_SC = A_WPA + 4 * D
        A_MB = A_SC + 2 * T
        A_END = A_MB + T
        arena = sb("arena", [128, A_END], BF16)
        wtok = arena[:, A_WTOK:A_WFEAT].rearrange("p (k c) -> p k c", k=KC)
        wfeat = arena[:, A_WFEAT:A_WOUT].rearrange("p (k c) -> p k c", k=KC)
        wout = arena[:, A_WOUT:A_WPP].rearrange("p (k c) -> p k c", k=KC)
        wpp = arena[:, A_WPP:A_WPA].rearrange("p (k c) -> p k c", k=4)
        wpa = arena[:, A_WPA:A_SC].rearrange("p (k c) -> p k c", k=4)
        scores = arena[:, A_SC:A_MB].bitcast(F32)
        mb = arena[:, A_MB:A_END]

        ident = sb("ident", [128, 128], BF16)
        i4 = sb("i4", [128, 512], BF16)
        band = sb("band", [128, 12, 128], BF16)
        cbias = sb("cbias", [128, 128], F32)
        pow2 = sb("pow2", [128, NBIS + 1], F32)
        lncol = sb("lncol", [128, 4, KC], F32)
        epst = sb("epst", [128, 1], F32)
        eps2t = sb("eps2t", [128, 1], F32)
        ones1 = sb("ones1", [1, 128], BF16)
        bgate = sb("bgate", [1, 2048], BF16)
        poolw = sb("poolw", [128, 4, 128], BF16)
        pscale = sb("pscale", [128, 4], F32)
        wr = sb("wr", [128, KC, 36], BF16)
        brt = sb("brt", [128, 36], F32)
        lng = sb("lng", [128, D], F32)
        lnb = sb("lnb", [128, D], F32)
        ln1g = sb("ln1g", [128, D], F32)
        ln1b = sb("ln1b", [128, D], F32)
        htok = [sb("htok%d" % i, [128, D], F32) for i in range(2)]
        hbf = sb("hbf", [128, D], BF16)
        hT = sb("hT", [128, KC, 128], BF16)
        stats = sb("stats", [128, 2, 6], F32)
        mv = sb("mv", [128, 2], F32)
        rstd = sb("rstd", [128, 1], F32)
        mv8 = sb("mv8", [128, 8, 2], F32)
        rs8 = sb("rs8", [128, 16], F32)
        nmr = sb("nmr", [128, 1], F32)
        upool = [sb("upool%d" % i, [128, 512], BF16) for i in range(2)]
        gates = [sb("gates%d" % i, [128, 2048], BF16) for i in range(2)]
        kT = sb("kT", [64, T], BF16)
        ikT4 = sb("ikT4", [128, T], BF16)
        vaug = sb("vaug", [128, NT, 65], BF16)
        qT = [sb("qT%d" % i, [64, 8 * 128], BF16) for i in range(2)]
        iqT = sb("iqT", [128, 3, 128], BF16)
        iwt = sb("iwt", [128, 8], F32)
        NRL = 4
        rl = [sb("rl%d" % i, [128, 512], F32) for i in range(NRL)]
        junk1 = sb("junk1", [128, 1], BF16)
        junk = junk1[:, 0:1].to_broadcast([128, T])
        junk2t = sb("junk2", [128, 1], BF16)
        junk2 = junk2t[:, 0:1].to_broadcast([128, T])
        sga = sb("sga", [128, 1], F32)
        mabs = sb("mabs", [128, 1], F32)
        wk = sb("wk", [128, NBIS + 1], F32)
        tcur = sb("tcur", [128, 1], F32)
        cnt = sb("cnt", [128, 1], F32)
        sgn = sb("sgn", [128, 1], F32)
        thr = sb("thr", [128, 1], F32)
        PT0 = sb("PT0", [128, 1024], BF16)
        PT = [PT0, PT0]
        rden = sb("rden", [128, 8], F32)
        hbf2 = sb("hbf2", [128, D], BF16)
        hT2 = sb("hT2", [128, KC, 128], BF16)
        ao = hbf2[:, 0:512]
        aoT = hT2[:, 0:4, :]
        deltaT = sb("deltaT", [128, 4, 128], BF16)
        poT = [sb("poT%d" % i, [128, 4, 128], BF16) for i in range(2)]
        mg = hbf2
        mgT = hT2
        h2bf = hbf2
        h2T = hT2
        rt = sb("rt", [128, 36], F32)
        rsm = sb("rsm", [128, 16], F32)
        m8 = sb("m8", [128, 8], F32)
        g1t = sb("g1t", [128, 32], F32)
        g2t = sb("g2t", [128, 32], F32)
        gtall = sb("gtall", [128, NT, 32], F32)

        psum = [es.enter_context(nc.psum_tensor("ps%d" % i, [128, 512], F32)) for i in range(8)]
        pbuf = [Buf() for _ in range(8)]
        gen = PsumPool([3, 4, 5])
        gen_idx = PsumPool([0, 1, 2])

        BW = bufof("w")
        P.dma("sp", ident[:], ident_d, writes=[BW], acc=True, stream="c")
        P.dma("sp", i4[:], i4_d, writes=[BW], acc=True, stream="c")
        P.dma("sp", band[:].rearrange("p a t -> p (a t)"), band_d, writes=[BW], acc=True, stream="c")
        P.dma("sp", cbias[:], cbias_d, writes=[BW], acc=True, stream="c")
        P.dma("sp", pow2[:], pow2_d, writes=[BW], acc=True, stream="c")
        P.dma("sp", pscale[:], pscale_d, writes=[BW], acc=True, stream="c")
        P.dma("sp", brt[:], br_d, writes=[BW], acc=True, stream="c")
        for j, dd in enumerate((lngc_d, lnbc_d, ln1gc_d, ln1bc_d)):
            P.dma("sp", lncol[:, j, :], dd, writes=[BW], acc=True, stream="c")
        P.dma("sp", lng[:], lng_d, writes=[bufof("lng")], stream="lg")
        P.dma("sp", lnb[:], lnb_d, writes=[bufof("lnb")], stream="lb")
        P.dma("sp", ln1g[:], ln1g_d, writes=[BW], acc=True, stream="c")
        P.dma("sp", ln1b[:], ln1b_d, writes=[BW], acc=True, stream="c")
        P.op("dve", lambda eng: eng.memset(epst[:], EPS), writes=[BW], acc=True)
        P.op("dve", lambda eng: eng.memset(eps2t[:], EPS / (ALPHA * ALPHA)), writes=[BW], acc=True)
        P.op("dve", lambda eng: eng.memset(ones1[:], 1.0), writes=[BW], acc=True)
        P.op("pool", lambda eng: eng.memset(vaug[:].rearrange("p a c -> p (a c)"), 1.0), writes=[bufof("vaug")])

        cast_n = [0]

        last_cast = {}

        def stage_cast(src, dst, rows, cols):
            for c0 in range(0, cols, 1024):
                c1 = min(cols, c0 + 1024)
                n = cast_n[0]
                cast_n[0] += 1
                j = n % 4
                s_ = scores[:, j * 1024:(j + 1) * 1024]
                sbf = bufof("stgS%d" % j)
                P.dma("sp", s_[0:rows, 0:c1 - c0], src[:, c0:c1], writes=[sbf], stream="w%d" % j)
                e = ("act", "dve", "pool", "act")[n % 4]
                if e == "act":
                    t = P.op("act", lambda eng, d=dst, s_=s_, c0=c0, c1=c1: eng.copy(out=d[:, c0:c1], in_=s_[0:rows, 0:c1 - c0]),
                             reads=[sbf], writes=[BW], acc=True)
                else:
                    t = P.op(e, lambda eng, d=dst, s_=s_, c0=c0, c1=c1: eng.tensor_copy(out=d[:, c0:c1], in_=s_[0:rows, 0:c1 - c0]),
                             reads=[sbf], writes=[BW], acc=True)
                last_cast[e] = t

        for kc in range(KC):
            stage_cast(wtok_d[:, kc, :], wtok[:, kc, :], 128, CTOK)
            stage_cast(wfeat_d[:, kc, :], wfeat[:, kc, :], 128, CFEAT)
        for g in range(4):
            stage_cast(wpp_d[:, g, :], wpp[:, g, :], 128, D)
            stage_cast(wpa_d[:, g, :], wpa[:, g, :], 128, D)
            stage_cast(poolw_d[:, g, :], poolw[:, g, :], 128, 128)
        for kc in range(KC):
            stage_cast(wout_d[:, kc, :], wout[:, kc, :], 128, D)
            stage_cast(wr_d[:, kc, :], wr[:, kc, :], 128, 36)
        stage_cast(bgate_d, bgate[:], 1, 2048)
        for t in last_cast.values():
            bufof("scores").r[id(t.sem)] = t

        def layer_norm(src, dst, gt, bt, srcbuf, dstbuf, gbufs, bf=None, bfbuf=None):
            for c in range(2):
                P.op("dve", lambda eng, c=c: eng.bn_stats(out=stats[:, c, :], in_=src[:, c * 512:(c + 1) * 512]),
                     reads=[srcbuf], writes=[bufof("stats")], acc=(c > 0))
            P.op("dve", lambda eng: eng.bn_aggr(out=mv[:], in_=stats[:].rearrange("p a s -> p (a s)")),
                 reads=[bufof("stats")], writes=[bufof("mv")])
            P.op("act", lambda eng: eng.activation(out=rstd[:], in_=mv[:, 1:2], func=AF.Sqrt, bias=epst[:], scale=1.0),
                 reads=[bufof("mv"), BW], writes=[bufof("rstd")])
            P.op("dve", lambda eng: eng.reciprocal(out=rstd[:], in_=rstd[:]), writes=[bufof("rstd")])
            P.op("dve", lambda eng: eng.scalar_tensor_tensor(out=nmr[:], in0=mv[:, 0:1], scalar=-1.0, in1=rstd[:],
                                                             op0=ALU.mult, op1=ALU.mult),
                 reads=[bufof("mv"), bufof("rstd")], writes=[bufof("nmr")])
            if bf is not None:
                P.op("act", lambda eng: eng.activation(out=bf[:], in_=src[:], func=AF.Identity, bias=nmr[:], scale=rstd[:]),
                     reads=[srcbuf, bufof("rstd"), bufof("nmr")], writes=[bfbuf])
            P.op("act", lambda eng: eng.activation(out=dst[:], in_=src[:], func=AF.Identity, bias=nmr[:], scale=rstd[:]),
                 reads=[srcbuf, bufof("rstd"), bufof("nmr")], writes=[dstbuf])
            P.op("pool", lambda eng: eng.tensor_mul(out=dst[:], in0=dst[:], in1=gt[:]), reads=gbufs, writes=[dstbuf])
            P.op("pool", lambda eng: eng.tensor_add(out=dst[:], in0=dst[:], in1=bt[:]), reads=gbufs, writes=[dstbuf])

        def transpose_to(src_bf, nblk, dst_flat, srcbuf, dstbuf, gb=None):
            pb = gen.get()
            pT = psum[pb][:].bitcast(BF16)
            for kc in range(nblk):
                P.op("pe", lambda eng, kc=kc, pT=pT: eng.transpose(out=pT[:, kc * 128:(kc + 1) * 128],
                                                                   in_=src_bf[:, kc * 128:(kc + 1) * 128], identity=ident[:]),
                     reads=[srcbuf, BW], writes=[pbuf[pb]], acc=(kc > 0))
            if gb is None:
                P.op("dve", lambda eng, pT=pT: eng.tensor_copy(out=dst_flat, in_=pT[:, 0:nblk * 128]),
                     reads=[pbuf[pb]], writes=[dstbuf])
            else:
                for kc in range(nblk):
                    P.op("dve", lambda eng, pT=pT, kc=kc: eng.tensor_scalar(
                        out=dst_flat[:, kc * 128:(kc + 1) * 128], in0=pT[:, kc * 128:(kc + 1) * 128],
                        scalar1=lncol[:, gb, kc:kc + 1], scalar2=lncol[:, gb + 1, kc:kc + 1], op0=ALU.mult, op1=ALU.add),
                        reads=[pbuf[pb], BW], writes=[dstbuf], acc=(kc > 0))

        final = []
        HB, HT = bufof("hbf"), bufof("hT")
        SC, MB = bufof("scores"), bufof("mb")

        def dbg_store(name, i, src, srcbuf, width):
            if name in dbg_d:
                final.append(P.dma("pool", dbg_d[name][i * 128:(i + 1) * 128, 0:width], src, reads=[srcbuf], stream="o"))

        def advance(g, n=1):
            if g is None:
                return
            for _ in range(n):
                try:
                    next(g)
                except StopIteration:
                    return

        def drain(g):
            if g is None:
                return
            for _ in g:
                pass

        def stage_X(i, g3=None, g2=None):
            H = bufof("htok0")
            P.dma("sp", htok[0][:], x_d[i * 128:(i + 1) * 128, :], writes=[H], stream="x0")
            layer_norm(htok[0], htok[0], lng, lnb, H, H, [bufof("lng"), bufof("lnb")], bf=hbf, bfbuf=HB)
            P.dma("pool", hscr_d[i * 128:(i + 1) * 128, :], htok[0][:], reads=[H], writes=[bufof("hscr%d" % (i % 4))], stream="hs%d" % (i % 4))
            transpose_to(hbf, KC, hT[:].rearrange("p k t -> p (k t)"), HB, HT, gb=0)
            pb = gen.get()
            for kc in range(KC):
                P.op("pe", lambda eng, kc=kc, pb=pb: eng.matmul(psum[pb][:, 0:72], lhsT=hT[:, kc, :], rhs=wtok[:, kc, 2560:2632],
                                                                start=(kc == 0), stop=(kc == KC - 1)),
                     reads=[HT, BW], writes=[pbuf[pb]], acc=(kc > 0))
            P.op("dve", lambda eng, pb=pb: eng.tensor_copy(out=vaug[:, i, 0:64], in_=psum[pb][:, 0:64]),
                 reads=[pbuf[pb]], writes=[bufof("vaug")], acc=True)
            P.op("dve", lambda eng, pb=pb: eng.tensor_copy(out=iwt[:], in_=psum[pb][:, 64:72]),
                 reads=[pbuf[pb]], writes=[bufof("iwt")])
            pb = gen.get()
            fm = ((512, 64), (576, 96), (672, 96), (768, 64))
            for j, (c0, m) in enumerate(fm):
                for kc in range(KC):
                    P.op("pe", lambda eng, kc=kc, pb=pb, j=j, c0=c0, m=m: eng.matmul(
                        psum[pb][0:m, j * 128:(j + 1) * 128], lhsT=wfeat[:, kc, c0:c0 + m], rhs=hT[:, kc, :],
                        start=(kc == 0), stop=(kc == KC - 1)),
                        reads=[HT, BW], writes=[pbuf[pb]], acc=(kc > 0 or j > 0))
            P.op("dve", lambda eng, pb=pb: eng.tensor_copy(out=kT[:, i * 128:(i + 1) * 128], in_=psum[pb][0:64, 0:128]),
                 reads=[pbuf[pb]], writes=[bufof("kT")], acc=True)
            P.op("dve", lambda eng, pb=pb: eng.tensor_copy(out=iqT[0:96, :, :].rearrange("p a t -> p (a t)"), in_=psum[pb][0:96, 128:512]),
                 reads=[pbuf[pb]], writes=[bufof("iqT")])
            pb = gen.get()
            for kc in range(KC):
                P.op("pe", lambda eng, kc=kc, pb=pb: eng.matmul(psum[pb][0:96, 0:128], lhsT=wfeat[:, kc, 832:928], rhs=hT[:, kc, :],
                                                                start=(kc == 0), stop=(kc == KC - 1)),
                     reads=[HT, BW], writes=[pbuf[pb]], acc=(kc > 0))
            P.op("act", lambda eng, pb=pb: eng.copy(out=ikT4[0:96, i * 128:(i + 1) * 128], in_=psum[pb][0:96, 0:128]),
                 reads=[pbuf[pb]], writes=[bufof("ikT4")], acc=True)
            S = 128 * (i + 1)
            nch = (S + 511) // 512
            nrl = 0
            n_it = 8 * nch
            cx = i // 3
            every2 = max(1, n_it // (cx + 1))
            advance(g3, 1)
            for c in range(nch):
                w = min(512, S - c * 512)
                for h in range(8):
                    if nrl % 3 == 2:
                        advance(g3, 1)
                    if cx > 0 and nrl % every2 == every2 - 1:
                        advance(g2, 1)
                    pb = gen_idx.get()
                    hp, hc = h % 3, h // 3
                    P.op("pe", lambda eng, pb=pb, hp=hp, hc=hc, c=c, w=w: eng.matmul(
                        psum[pb][:, 0:w], lhsT=iqT[hp * 32:(hp + 1) * 32, hc, :], rhs=ikT4[hp * 32:(hp + 1) * 32, c * 512:c * 512 + w],
                        start=True, stop=True),
                        reads=[bufof("iqT"), bufof("ikT4")], writes=[pbuf[pb]])
                    r = nrl % NRL
                    nrl += 1
                    RL = bufof("rl%d" % r)
                    P.op("act", lambda eng, pb=pb, r=r, w=w: eng.activation(out=rl[r][:, 0:w], in_=psum[pb][:, 0:w], func=AF.Relu),
                         reads=[pbuf[pb]], writes=[RL])
                    if h == 0:
                        P.op("dve", lambda eng, r=r, c=c, w=w: eng.tensor_scalar(
                            out=scores[:, c * 512:c * 512 + w], in0=rl[r][:, 0:w], scalar1=iwt[:, 0:1], scalar2=None, op0=ALU.mult),
                            reads=[RL, bufof("iwt")], writes=[SC], acc=(c > 0))
                    else:
                        P.op("dve", lambda eng, r=r, c=c, w=w, h=h: eng.scalar_tensor_tensor(
                            out=scores[:, c * 512:c * 512 + w], in0=rl[r][:, 0:w], scalar=iwt[:, h:h + 1],
                            in1=scores[:, c * 512:c * 512 + w], op0=ALU.mult, op1=ALU.add),
                            reads=[RL, bufof("iwt")], writes=[SC], acc=True)

        def stage_Y(i, g3=None, g2=None, n2=2):
            S = 128 * (i + 1)
            P.op("dve", lambda eng: eng.tensor_reduce(out=mabs[:], in_=scores[:, 0:S], axis=AX.X, op=ALU.max,
                                                      apply_absolute_value=True),
                 reads=[SC], writes=[bufof("mabs")])
            P.op("dve", lambda eng: eng.tensor_tensor(out=scores[:, i * 128:(i + 1) * 128], in0=scores[:, i * 128:(i + 1) * 128],
                                                      in1=cbias[:], op=ALU.add),
                 reads=[BW], writes=[SC])
            P.op("dve", lambda eng: eng.tensor_scalar(out=mabs[:], in0=mabs[:], scalar1=1.001, scalar2=1e-20, op0=ALU.mult, op1=ALU.add),
                 writes=[bufof("mabs")])
            P.op("dve", lambda eng: eng.tensor_scalar(out=wk[:], in0=pow2[:], scalar1=mabs[:, 0:1], scalar2=None, op0=ALU.mult),
                 reads=[bufof("mabs"), BW], writes=[bufof("wk")])
            P.op("dve", lambda eng: eng.memset(tcur[:], 0.0), writes=[bufof("tcur")])
            JK = bufof("junk")
            split = (S >= ACT_SPLIT_MIN)
            Sd = (S // 2) if split else S
            na = S - Sd
            for k in range(NBIS if S > NSEL else 0):
                P.op("dve", lambda eng: eng.tensor_scalar(out=junk[:, 0:Sd], in0=scores[:, 0:Sd], scalar1=tcur[:, 0:1], scalar2=0.0,
                                                          op0=ALU.is_ge, op1=ALU.add, accum_out=cnt[:]),
                     reads=[SC, bufof("tcur")], writes=[JK, bufof("cnt")])
                if split:
                    P.op("act", lambda eng: eng.activation(out=junk2[:, 0:na], in_=scores[:, Sd:S], func=AF.Sign, bias=tcur[:, 0:1],
                                                           scale=-1.0, accum_out=sga[:]),
                         reads=[SC, bufof("tcur")], writes=[bufof("junk2"), bufof("sga")])
                    P.op("dve", lambda eng: eng.scalar_tensor_tensor(out=cnt[:], in0=cnt[:], scalar=2.0, in1=sga[:],
                                                                     op0=ALU.mult, op1=ALU.subtract),
                         reads=[bufof("sga")], writes=[bufof("cnt")])
                    P.op("dve", lambda eng: eng.tensor_scalar(out=sgn[:], in0=cnt[:], scalar1=float(2 * NSEL - na), scalar2=-0.5,
                                                              op0=ALU.is_ge, op1=ALU.add),
                         reads=[bufof("cnt")], writes=[bufof("sgn")])
                else:
                    P.op("dve", lambda eng: eng.tensor_scalar(out=sgn[:], in0=cnt[:], scalar1=float(NSEL), scalar2=-0.5,
                                                              op0=ALU.is_ge, op1=ALU.add),
                         reads=[bufof("cnt")], writes=[bufof("sgn")])
                P.op("dve", lambda eng, k=k: eng.scalar_tensor_tensor(out=tcur[:], in0=sgn[:], scalar=wk[:, k:k + 1], in1=tcur[:],
                                                                      op0=ALU.mult, op1=ALU.add),
                     reads=[bufof("sgn"), bufof("wk")], writes=[bufof("tcur")])
                advance(g3, 1)
                advance(g2, n2)
            drain(g2)
            if S > NSEL:
                P.op("dve", lambda eng: eng.scalar_tensor_tensor(out=thr[:], in0=wk[:, NBIS:NBIS + 1], scalar=-1.0, in1=tcur[:],
                                                                 op0=ALU.mult, op1=ALU.add),
                     reads=[bufof("wk"), bufof("tcur")], writes=[bufof("thr")])
            else:
                P.op("dve", lambda eng: eng.tensor_scalar(out=thr[:], in0=wk[:, 0:1], scalar1=-1.0, scalar2=None, op0=ALU.mult),
                     reads=[bufof("wk")], writes=[bufof("thr")])
            P.op("dve", lambda eng: eng.tensor_scalar(out=mb[:, 0:S], in0=scores[:, 0:S], scalar1=thr[:, 0:1], scalar2=NEG,
                                                      op0=ALU.is_lt, op1=ALU.mult),
                 reads=[SC, bufof("thr")], writes=[MB])
            dbg_store("thr", i, thr[:], bufof("thr"), 1)
            drain(g3)
            drain(g2)

        def stage_W(i):
            ub = i % 2
            UP, UPP = bufof("upool%d" % ub), bufof("upool%d" % (1 - ub))
            GT, QT, POT = bufof("gates%d" % ub), bufof("qT%d" % ub), bufof("poT%d" % ub)
            pb = gen.get()
            for kc in range(KC):
                P.op("pe", lambda eng, kc=kc, pb=pb: eng.matmul(psum[pb][:], lhsT=hT[:, kc, :], rhs=wtok[:, kc, 0:512],
                                                                start=(kc == 0), stop=(kc == KC - 1)),
                     reads=[HT, BW], writes=[pbuf[pb]], acc=(kc > 0))
            P.op("act", lambda eng, pb=pb: eng.copy(out=upool[ub][:], in_=psum[pb][:]), reads=[pbuf[pb]], writes=[UP])
            for j in range(4):
                pb = gen.get()
                for kc in range(KC):
                    P.op("pe", lambda eng, kc=kc, pb=pb, j=j: eng.matmul(psum[pb][:], lhsT=hT[:, kc, :],
                                                                         rhs=wtok[:, kc, 512 + j * 512:1024 + j * 512],
                                                                         start=(kc == 0), stop=False),
                         reads=[HT, BW], writes=[pbuf[pb]], acc=(kc > 0))
                P.op("pe", lambda eng, pb=pb, j=j: eng.matmul(psum[pb][:], lhsT=ones1[0:1, :], rhs=bgate[0:1, j * 512:(j + 1) * 512],
                                                              start=False, stop=True),
                     reads=[BW], writes=[pbuf[pb]], acc=True)
                P.op("act", lambda eng, pb=pb, j=j: eng.activation(out=gates[ub][:, j * 512:(j + 1) * 512], in_=psum[pb][:], func=AF.Sigmoid),
                     reads=[pbuf[pb]], writes=[GT], acc=(j > 0))
            for half in range(2):
                pb = gen.get()
                for hh in range(4):
                    h = half * 4 + hh
                    for kc in range(KC):
                        P.op("pe", lambda eng, kc=kc, pb=pb, hh=hh, h=h: eng.matmul(
                            psum[pb][0:64, hh * 128:(hh + 1) * 128], lhsT=wfeat[:, kc, h * 64:(h + 1) * 64], rhs=hT[:, kc, :],
                            start=(kc == 0), stop=(kc == KC - 1)),
                            reads=[HT, BW], writes=[pbuf[pb]], acc=(kc > 0 or hh > 0))
                P.op("act", lambda eng, pb=pb, half=half: eng.copy(out=qT[ub][:, half * 512:(half + 1) * 512], in_=psum[pb][0:64, :]),
                     reads=[pbuf[pb]], writes=[QT], acc=(half > 0))
            pb = gen.get()
            for g in range(4):
                P.op("pe", lambda eng, pb=pb, g=g: eng.matmul(
                    psum[pb][:, g * 128:(g + 1) * 128], lhsT=upool[ub][:, g * 128:(g + 1) * 128],
                    rhs=band[:, (8 + g) if i == 0 else g, :], start=True, stop=(i == 0)),
                    reads=[UP, BW], writes=[pbuf[pb]], acc=(g > 0))
                if i > 0:
                    P.op("pe", lambda eng, pb=pb, g=g: eng.matmul(
                        psum[pb][:, g * 128:(g + 1) * 128], lhsT=upool[1 - ub][:, g * 128:(g + 1) * 128],
                        rhs=band[:, 4 + g, :], start=False, stop=True),
                        reads=[UPP, BW], writes=[pbuf[pb]], acc=True)
            P.op("act", lambda eng, pb=pb: eng.copy(out=deltaT[:].rearrange("p a t -> p (a t)"), in_=psum[pb][:]),
                 reads=[pbuf[pb]], writes=[bufof("deltaT")])
            pb = gen.get()
            for g in range(4):
                P.op("pe", lambda eng, pb=pb, g=g: eng.matmul(psum[pb][:, g * 128:(g + 1) * 128], lhsT=poolw[:, g, :],
                                                              rhs=deltaT[:, g, :], start=True, stop=True),
                     reads=[bufof("deltaT"), BW], writes=[pbuf[pb]], acc=(g > 0))
            for g in range(4):
                P.op("dve", lambda eng, pb=pb, g=g: eng.tensor_scalar(out=poT[ub][:, g, :], in0=psum[pb][:, g * 128:(g + 1) * 128],
                                                                      scalar1=pscale[:, g:g + 1], scalar2=None, op0=ALU.mult),
                     reads=[pbuf[pb], BW], writes=[POT], acc=(g > 0))

        def stage_S2(i):
            ub = i % 2
            QT = bufof("qT%d" % ub)
            for c in range(i + 1):
                pr = c % 2
                PTB = bufof("PT0")
                for half in range(2):
                    pb = gen.get()
                    P.op("pe", lambda eng, pb=pb, c=c, half=half: eng.matmul(
                        psum[pb][:], lhsT=kT[:, c * 128:(c + 1) * 128], rhs=qT[ub][:, half * 512:(half + 1) * 512], start=True, stop=False),
                        reads=[bufof("kT"), QT], writes=[pbuf[pb]])
                    P.op("pe", lambda eng, pb=pb, c=c: eng.matmul(
                        psum[pb][:], lhsT=mb[:, c * 128:(c + 1) * 128], rhs=i4[:], start=False, stop=True),
                        reads=[MB, BW], writes=[pbuf[pb]], acc=True)
                    P.op("act", lambda eng, pb=pb, pr=pr, half=half: eng.activation(
                        out=PT[pr][:, half * 512:(half + 1) * 512], in_=psum[pb][:], func=AF.Exp, scale=SM_SCALE),
                        reads=[pbuf[pb]], writes=[PTB], acc=(half > 0))
                for h in range(8):
                    ob = 6 + h // 4
                    P.op("pe", lambda eng, ob=ob, h=h, pr=pr, c=c: eng.matmul(
                        psum[ob][:, (h % 4) * 65:(h % 4) * 65 + 65], lhsT=PT[pr][:, h * 128:(h + 1) * 128], rhs=vaug[:, c, :],
                        start=(c == 0 and h % 4 == 0), stop=(c == i and h % 4 == 3)),
                        reads=[PTB, bufof("vaug")], writes=[pbuf[ob]], acc=not (c == 0 and h % 4 == 0))
                yield

        def stage_S3(i):
            ub = i % 2
            xb = 1
            H = bufof("htok1")
            GT, POT = bufof("gates%d" % ub), bufof("poT%d" % ub)
            HB, HT = bufof("hbf2"), bufof("hT2")
            P.dma("sp", htok[1][:], hscr_d[i * 128:(i + 1) * 128, :], reads=[bufof("hscr%d" % (i % 4))], writes=[H], stream="hl")
            for hb in range(2):
                ob = 6 + hb
                ov = psum[ob][:, 0:260].rearrange("p (h e) -> p h e", e=65)
                P.op("dve", lambda eng, ov=ov, hb=hb: eng.reciprocal(out=rden[:, hb * 4:(hb + 1) * 4], in_=ov[:, :, 64]),
                     reads=[pbuf[ob]], writes=[bufof("rden")], acc=(hb > 0))
                P.op("dve", lambda eng, ov=ov, hb=hb: eng.tensor_tensor(
                    out=ao[:, hb * 256:(hb + 1) * 256].rearrange("p (h e) -> p h e", e=64), in0=ov[:, :, 0:64],
                    in1=rden[:, hb * 4:(hb + 1) * 4].unsqueeze(2).to_broadcast([128, 4, 64]), op=ALU.mult),
                    reads=[pbuf[ob], bufof("rden")], writes=[HB], acc=(hb > 0))
            yield
            if "ao" in dbg_d:
                P.op("dve", lambda eng: eng.tensor_copy(out=rl[0][:], in_=ao), reads=[HB], writes=[bufof("rl0")])
                dbg_store("ao", i, rl[0][:], bufof("rl0"), 512)
            if "po" in dbg_d:
                P.op("dve", lambda eng: eng.tensor_copy(out=rl[1][:], in_=poT[ub][:].rearrange("p a t -> p (a t)")),
                     reads=[POT], writes=[bufof("rl1")])
                dbg_store("po", i, rl[1][:], bufof("rl1"), 512)
            transpose_to(ao, 4, aoT.rearrange("p a t -> p (a t)"), HB, HT)
            yield
            for n in range(2):
                pbp = gen.get()
                for g in range(4):
                    P.op("pe", lambda eng, pbp=pbp, g=g, n=n: eng.matmul(psum[pbp][:], lhsT=poT[ub][:, g, :], rhs=wpp[:, g, n * 512:(n + 1) * 512],
                                                                         start=(g == 0), stop=(g == 3)),
                         reads=[POT, BW], writes=[pbuf[pbp]], acc=(g > 0))
                pba = gen.get()
                for g in range(4):
                    P.op("pe", lambda eng, pba=pba, g=g, n=n: eng.matmul(psum[pba][:], lhsT=aoT[:, g, :], rhs=wpa[:, g, n * 512:(n + 1) * 512],
                                                                         start=(g == 0), stop=(g == 3)),
                         reads=[HT, BW], writes=[pbuf[pba]], acc=(g > 0))
                P.op("dve", lambda eng, pbp=pbp, n=n: eng.tensor_tensor(out=rl[0][:], in0=gates[ub][:, n * 512:(n + 1) * 512], in1=psum[pbp][:], op=ALU.mult),
                     reads=[GT, pbuf[pbp]], writes=[bufof("rl0")])
                P.op("dve", lambda eng, pba=pba, n=n: eng.tensor_tensor(out=rl[1][:], in0=gates[ub][:, 1024 + n * 512:1536 + n * 512], in1=psum[pba][:], op=ALU.mult),
                     reads=[GT, pbuf[pba]], writes=[bufof("rl1")])
                P.op("pool", lambda eng, n=n: eng.tensor_add(out=mg[:, n * 512:(n + 1) * 512], in0=rl[0][:], in1=rl[1][:]),
                     reads=[bufof("rl0"), bufof("rl1")], writes=[HB], acc=(n > 0))
                yield
            transpose_to(mg, KC, mgT[:].rearrange("p k t -> p (k t)"), HB, HT)
            yield
            for n in range(2):
                pb = gen.get()
                for kc in range(KC):
                    P.op("pe", lambda eng, pb=pb, kc=kc, n=n: eng.matmul(psum[pb][:], lhsT=mgT[:, kc, :], rhs=wout[:, kc, n * 512:(n + 1) * 512],
                                                                         start=(kc == 0), stop=(kc == KC - 1)),
                         reads=[HT, BW], writes=[pbuf[pb]], acc=(kc > 0))
                P.op("dve", lambda eng, pb=pb, n=n: eng.scalar_tensor_tensor(
                    out=htok[xb][:, n * 512:(n + 1) * 512], in0=htok[xb][:, n * 512:(n + 1) * 512], scalar=ALPHA, in1=psum[pb][:],
                    op0=ALU.mult, op1=ALU.add),
                    reads=[pbuf[pb]], writes=[H])
                yield
            h2 = htok[xb]
            layer_norm(h2, h2, ln1g, ln1b, H, H, [BW])
            dbg_store("h1", i, h2[:], H, D)
            if do_moe:
                P.dma("pool", h2_d[i * 128:(i + 1) * 128, :], h2[:], reads=[H], writes=[bufof("h2_d")], acc=True, stream="s")
            P.op("act", lambda eng: eng.copy(out=h2bf[:], in_=h2[:]), reads=[H], writes=[HB])
            transpose_to(h2bf, KC, h2T[:].rearrange("p k t -> p (k t)"), HB, HT)
            yield
            if do_moe:
                P.dma("pool", h2T_d[:, :, i * 128:(i + 1) * 128], h2T[:], reads=[HT], writes=[bufof("h2T_d")], acc=True, stream="s")
            pb = gen.get()
            for kc in range(KC):
                P.op("pe", lambda eng, pb=pb, kc=kc: eng.matmul(psum[pb][:, 0:36], lhsT=h2T[:, kc, :], rhs=wr[:, kc, :],
                                                                start=(kc == 0), stop=(kc == KC - 1)),
                     reads=[HT, BW], writes=[pbuf[pb]], acc=(kc > 0))
            RT, RS = bufof("rt"), bufof("rsm")
            P.op("dve", lambda eng, pb=pb: eng.tensor_tensor(out=rt[:], in0=psum[pb][:, 0:36], in1=brt[:], op=ALU.add),
                 reads=[pbuf[pb], BW], writes=[RT])
            P.op("dve", lambda eng: eng.tensor_reduce(out=rsm[:, 0:1], in_=rt[:, 0:4], axis=AX.X, op=ALU.max), reads=[RT], writes=[RS])
            P.op("dve", lambda eng: eng.tensor_scalar(out=rsm[:, 1:2], in0=rsm[:, 0:1], scalar1=-1.0, scalar2=None, op0=ALU.mult), writes=[RS])
            yield
            P.op("act", lambda eng: eng.activation(out=rsm[:, 12:16], in_=rt[:, 0:4], func=AF.Exp, bias=rsm[:, 1:2], scale=1.0,
                                                   accum_out=rsm[:, 2:3]), reads=[RT], writes=[RS])
            P.op("dve", lambda eng: eng.reciprocal(out=rsm[:, 3:4], in_=rsm[:, 2:3]), writes=[RS])
            P.op("dve", lambda eng: eng.tensor_scalar(out=rsm[:, 4:8], in0=rt[:, 0:4], scalar1=rsm[:, 0:1], scalar2=-1e30,
                                                      op0=ALU.is_lt, op1=ALU.mult), reads=[RT], writes=[RS])
            P.op("dve", lambda eng: eng.tensor_tensor(out=rt[:, 4:36].rearrange("p (g j) -> p g j", j=8),
                                                      in0=rt[:, 4:36].rearrange("p (g j) -> p g j", j=8),
                                                      in1=rsm[:, 4:8].unsqueeze(2).to_broadcast([128, 4, 8]), op=ALU.add),
                 reads=[RS], writes=[RT])
            P.op("dve", lambda eng: eng.max(out=m8[:], in_=rt[:, 4:36]), reads=[RT], writes=[bufof("m8")])
            P.op("dve", lambda eng: eng.tensor_tensor(out=rsm[:, 8:9], in0=m8[:, 1:2], in1=m8[:, 0:1], op=ALU.subtract),
                 reads=[bufof("m8")], writes=[RS])
            yield
            P.op("act", lambda eng: eng.activation(out=rsm[:, 9:10], in_=rsm[:, 8:9], func=AF.Exp), writes=[RS])
            P.op("dve", lambda eng: eng.tensor_scalar(out=rsm[:, 10:11], in0=rsm[:, 9:10], scalar1=1.0, scalar2=None, op0=ALU.add), writes=[RS])
            P.op("dve", lambda eng: eng.reciprocal(out=rsm[:, 10:11], in_=rsm[:, 10:11]), writes=[RS])
            P.op("dve", lambda eng: eng.scalar_tensor_tensor(out=rsm[:, 10:11], in0=rsm[:, 10:11], scalar=1.0 / ALPHA, in1=rsm[:, 3:4],
                                                             op0=ALU.mult, op1=ALU.mult), writes=[RS])
            P.op("dve", lambda eng: eng.tensor_tensor(out=rsm[:, 11:12], in0=rsm[:, 10:11], in1=rsm[:, 9:10], op=ALU.mult), writes=[RS])
            P.op("dve", lambda eng: eng.tensor_scalar(out=g1t[:], in0=rt[:, 4:36], scalar1=m8[:, 0:1], scalar2=rsm[:, 10:11],
                                                      op0=ALU.is_equal, op1=ALU.mult), reads=[RT, RS, bufof("m8")], writes=[bufof("g1t")])
            P.op("dve", lambda eng: eng.tensor_scalar(out=g2t[:], in0=rt[:, 4:36], scalar1=m8[:, 1:2], scalar2=rsm[:, 11:12],
                                                      op0=ALU.is_equal, op1=ALU.mult), reads=[RT, RS, bufof("m8")], writes=[bufof("g2t")])
            P.op("dve", lambda eng: eng.tensor_tensor(out=gtall[:, i, :], in0=g1t[:], in1=g2t[:], op=ALU.add),
                 reads=[bufof("g1t"), bufof("g2t")], writes=[bufof("gtall")], acc=True)
            dbg_store("gm", i, gtall[:, i, :], bufof("gtall"), 32)
            if not do_moe:
                final.append(P.dma("pool", out_d[i * 128:(i + 1) * 128, :], h2[:], reads=[H], stream="o"))

        for it in range(ntiles + 2):
            g3 = stage_S3(it - 2) if it - 2 >= 0 else None
            g2 = stage_S2(it - 1) if 0 <= it - 1 < ntiles else None
            if it < ntiles:
                stage_X(it, g3, g2)
                rem = it - it // 3
                n2 = max(1, (rem + NBIS - 3) // (NBIS - 2))
                stage_Y(it, g3, g2, n2)
                stage_W(it)
            else:
                drain(g3)
                drain(g2)

        if do_moe:
            last = []
            for e, L in P.L.items():
                for it in reversed(L):
                    if isinstance(it, tuple):
                        last.append(it[1])
                        break
            BAR = Buf()
            for t in last:
                BAR.w[id(t.sem)] = t
            for nm in ("h2_d", "h2T_d"):
                for k, t in bufof(nm).w.items():
                    BAR.w[k] = t
            TB = 1024
            ntok = ntiles * 128
            nblk = (ntok + TB - 1) // TB
            o = 0
            wgb, wub, wdb, stgb = [], [], [], []
            for par in range(2):
                wgb.append(arena[:, o:o + 2048].rearrange("p (k f) -> p k f", k=KC)); o += 2048
                wub.append(arena[:, o:o + 2048].rearrange("p (k f) -> p k f", k=KC)); o += 2048
                wdb.append(arena[:, o:o + 2048].rearrange("p (k f) -> p k f", k=2)); o += 2048
            for j in range(3):
                stgb.append(arena[:, o:o + 4096].bitcast(F32)); o += 4096
            accv = arena[:, o:o + 16384].bitcast(F32).rearrange("p (a d) -> p a d", a=8); o += 16384
            hblk = arena[:, o:o + 8192].rearrange("p (k t) -> p k t", k=KC); o += 8192
            sgt, hid = [], []
            for j in range(2):
                sgt.append(arena[:, o:o + 512]); o += 512
            for j in range(3):
                hid.append(arena[:, o:o + 1024].rearrange("p (f t) -> p f t", f=2)); o += 1024
            assert o <= A_END, (o, A_END)
            P.dma("sp", lng[:], ln2g_d, reads=[BAR], writes=[bufof("lng")], stream="lg")
            P.dma("sp", lnb[:], ln2b_d, reads=[BAR], writes=[bufof("lnb")], stream="lb")

            items = []
            for blk in range(nblk):
                t0 = blk * TB
                tb = min(TB, ntok - t0)
                for e in range(nexp):
                    for sub in range(0, tb, 512):
                        items.append((blk, e, t0, sub, min(512, tb - sub)))
            eseq = []
            for it in items:
                if not eseq or eseq[-1] != (it[0], it[1]):
                    eseq.append((it[0], it[1]))
            item_j = []
            jj = -1
            prev = None
            for it in items:
                if (it[0], it[1]) != prev:
                    jj += 1
                    prev = (it[0], it[1])
                item_j.append(jj)
            srcs = (wg_d, wu_d, wd_d)

            def emit_stage(j):
                if j >= len(eseq):
                    return
                e = eseq[j][1]
                for m in range(3):
                    kk = 8 if m < 2 else 2
                    P.dma("sp", stgb[m][:].rearrange("p (k f) -> p k f", k=kk), srcs[m][e], reads=[BAR],
                          writes=[bufof("stgb%d" % m)], stream="ws%d" % m)

            def emit_cast(j):
                if j >= len(eseq):
                    return
                par = j % 2
                dsts = (wgb[par], wub[par], wdb[par])
                for m in range(3):
                    WB = bufof("wb%d_%d" % (m, par))
                    if m < 2:
                        P.op("act", lambda eng, d=dsts[m], m=m: eng.copy(out=d.rearrange("p k f -> p (k f)"), in_=stgb[m][:]),
                             reads=[bufof("stgb%d" % m), BAR], writes=[WB])
                    else:
                        P.op("dve", lambda eng, d=dsts[m], m=m: eng.tensor_copy(out=d.rearrange("p k f -> p (k f)"), in_=stgb[m][:]),
                             reads=[bufof("stgb%d" % m), BAR], writes=[WB])

            gu_banks = ((0, 1), (2, 3))
            gu_n = [0]
            d_n = [0]
            rl_n = [0]
            loaded_blk = [-1]

            def emit_GU(s, f):
                if s >= len(items):
                    return
                blk, e, t0, sub, sw = items[s]
                par = item_j[s] % 2
                hh_ = sub // 512
                HBK = bufof("hblk%d" % hh_)
                if f == 0 and e == 0:
                    P.dma("sp", hblk[:, :, sub:sub + sw], h2T_d[:, :, t0 + sub:t0 + sub + sw], reads=[BAR], writes=[HBK], stream="hb%d" % hh_)
                pg, pu = gu_banks[gu_n[0] % 2]
                sp_ = gu_n[0] % 2
                gu_n[0] += 1
                hb_ = s % 3
                WG, WU = bufof("wb0_%d" % par), bufof("wb1_%d" % par)
                for kc in range(KC):
                    P.op("pe", lambda eng, kc=kc: eng.matmul(psum[pg][:, 0:sw], lhsT=wgb[par][:, kc, f * 128:(f + 1) * 128],
                                                             rhs=hblk[:, kc, sub:sub + sw], start=(kc == 0), stop=(kc == KC - 1)),
                         reads=[WG, HBK, BAR], writes=[pbuf[pg]], acc=(kc > 0))
                for kc in range(KC):
                    P.op("pe", lambda eng, kc=kc: eng.matmul(psum[pu][:, 0:sw], lhsT=wub[par][:, kc, f * 128:(f + 1) * 128],
                                                             rhs=hblk[:, kc, sub:sub + sw], start=(kc == 0), stop=(kc == KC - 1)),
                         reads=[WU, HBK, BAR], writes=[pbuf[pu]], acc=(kc > 0))
                SG = bufof("sgt%d" % sp_)
                P.op("act", lambda eng: eng.activation(out=sgt[sp_][:, 0:sw], in_=psum[pg][:, 0:sw], func=AF.Silu),
                     reads=[pbuf[pg], BAR], writes=[SG])
                P.op("dve", lambda eng: eng.tensor_tensor(out=hid[hb_][:, f, 0:sw], in0=sgt[sp_][:, 0:sw], in1=psum[pu][:, 0:sw], op=ALU.mult),
                     reads=[SG, pbuf[pu], BAR], writes=[bufof("hid%d_%d" % (hb_, f))])

            def emit_D(s):
                blk, e, t0, sub, sw = items[s]
                par = item_j[s] % 2
                hb_ = s % 3
                WD = bufof("wb2_%d" % par)
                g = 0
                for tt in range(sw // 128):
                    ta = (sub // 128) + tt
                    tg = t0 // 128 + ta
                    for n in range(2):
                        pd = 4 + d_n[0] % 4
                        d_n[0] += 1
                        for f in range(2):
                            P.op("pe", lambda eng, f=f, tt=tt, n=n, pd=pd: eng.matmul(
                                psum[pd][:], lhsT=hid[hb_][:, f, tt * 128:(tt + 1) * 128], rhs=wdb[par][:, f, n * 512:(n + 1) * 512],
                                start=(f == 0), stop=(f == 1)),
                                reads=[bufof("hid%d_%d" % (hb_, f)), WD], writes=[pbuf[pd]], acc=(f > 0))
                        ACC = bufof("acc%d_%d" % (ta, n))
                        av = accv[:, ta, n * 512:(n + 1) * 512]
                        gsc = gtall[:, tg, e:e + 1]
                        if g not in (1, 4, 7):
                            P.op("dve", lambda eng, pd=pd, av=av, gsc=gsc: eng.scalar_tensor_tensor(out=av, in0=psum[pd][:], scalar=gsc, in1=av,
                                                                                                   op0=ALU.mult, op1=ALU.add),
                                 reads=[pbuf[pd], bufof("gtall"), BAR], writes=[ACC])
                        else:
                            r = rl_n[0] % NRL
                            rl_n[0] += 1
                            RL = bufof("rl%d" % r)
                            P.op("act", lambda eng, pd=pd, r=r, gsc=gsc: eng.activation(out=rl[r][:], in_=psum[pd][:], func=AF.Copy, scale=gsc),
                                 reads=[pbuf[pd], bufof("gtall"), BAR], writes=[RL])
                            P.op("pool", lambda eng, r=r, av=av: eng.tensor_add(out=av, in0=av, in1=rl[r][:]),
                                 reads=[RL, BAR], writes=[ACC])
                        g += 1

            def emit_acc_init(blk, ta):
                t0 = blk * TB
                if t0 + ta * 128 >= ntok:
                    return
                tg = t0 // 128 + ta
                ACC = [bufof("acc%d_0" % ta), bufof("acc%d_1" % ta)]
                P.dma("sp", accv[:, ta, :], h2_d[tg * 128:(tg + 1) * 128, :], reads=[BAR], writes=ACC, stream="ai%d" % ta)

            def emit_block_final(blk, half):
                t0 = blk * TB
                tb = min(TB, ntok - t0)
                ta0 = half * 4
                ta1 = min(tb // 128, ta0 + 4)
                ntt = ta1 - ta0
                if ntt <= 0:
                    return
                MV8, RS8 = bufof("mv8_%d" % half), bufof("rs8_%d" % half)
                for ta in range(ta0, ta1):
                    ACC0, ACC1 = bufof("acc%d_0" % ta), bufof("acc%d_1" % ta)
                    tg = t0 // 128 + ta
                    if "ffn" in dbg_d:
                        final.append(P.dma("pool", dbg_d["ffn"][tg * 128:(tg + 1) * 128, :], accv[:, ta, :], reads=[ACC0, ACC1], stream="o"))
                    src = accv[:, ta, :]
                    for c in range(2):
                        P.op("dve", lambda eng, c=c, src=src: eng.bn_stats(out=stats[:, c, :], in_=src[:, c * 512:(c + 1) * 512]),
                             reads=[ACC0 if c == 0 else ACC1], writes=[bufof("stats")], acc=(c > 0))
                    P.op("dve", lambda eng, ta=ta: eng.bn_aggr(out=mv8[:, ta, :], in_=stats[:].rearrange("p a s -> p (a s)")),
                         reads=[bufof("stats")], writes=[MV8], acc=(ta > ta0))
                P.op("act", lambda eng: eng.activation(out=rs8[:, ta0:ta1], in_=mv8[:, ta0:ta1, 1], func=AF.Sqrt, bias=eps2t[:], scale=1.0),
                     reads=[MV8, BW], writes=[RS8])
                P.op("dve", lambda eng: eng.reciprocal(out=rs8[:, ta0:ta1], in_=rs8[:, ta0:ta1]), writes=[RS8])
                P.op("dve", lambda eng: eng.scalar_tensor_tensor(out=rs8[:, 8 + ta0:8 + ta1], in0=mv8[:, ta0:ta1, 0], scalar=-1.0, in1=rs8[:, ta0:ta1],
                                                                 op0=ALU.mult, op1=ALU.mult),
                     reads=[MV8], writes=[RS8])
                for ta in range(ta0, ta1):
                    ACC0, ACC1 = bufof("acc%d_0" % ta), bufof("acc%d_1" % ta)
                    tg = t0 // 128 + ta
                    xb = ta % 2
                    HH = bufof("htok%d" % xb)
                    src = accv[:, ta, :]
                    P.op("act", lambda eng, xb=xb, src=src, ta=ta: eng.activation(out=htok[xb][:], in_=src, func=AF.Identity,
                                                                                 bias=rs8[:, 8 + ta:9 + ta], scale=rs8[:, ta:ta + 1]),
                         reads=[ACC0, ACC1, RS8], writes=[HH])
                    if blk + 1 < nblk:
                        emit_acc_init(blk + 1, ta)
                    P.op("dve", lambda eng, xb=xb: eng.tensor_mul(out=htok[xb][:], in0=htok[xb][:], in1=lng[:]),
                         reads=[bufof("lng")], writes=[HH])
                    P.op("dve", lambda eng, xb=xb: eng.tensor_add(out=htok[xb][:], in0=htok[xb][:], in1=lnb[:]),
                         reads=[bufof("lnb")], writes=[HH])
                    final.append(P.dma("pool", out_d[tg * 128:(tg + 1) * 128, :], htok[xb][:], reads=[HH], stream="o"))

            for ta in range(TB // 128):
                emit_acc_init(0, ta)
            emit_stage(0)
            emit_cast(0)
            emit_stage(1)
            emit_cast(1)
            emit_stage(2)
            emit_GU(0, 0)
            emit_GU(0, 1)
            emit_GU(1, 0)
            N = len(items)
            for s in range(N):
                emit_D(s)
                last_of_expert = (s == N - 1) or (item_j[s + 1] != item_j[s])
                if last_of_expert:
                    j = item_j[s]
                    emit_cast(j + 2)
                    emit_stage(j + 3)
                if items[s][1] == nexp - 1:
                    emit_block_final(items[s][0], items[s][3] // 512)
                emit_GU(s + 1, 1)
                emit_GU(s + 2, 0)

        fw = {}
        for t in final:
            k = id(t.sem)
            if k not in fw or fw[k].val < t.val:
                fw[k] = t
        P.finalize(list(fw.values()))
        print("ops traced:", P.nops, flush=True)
    return nc


def host_inputs(inputs):
    f = np.float32
    bf = ml_dtypes.bfloat16
    w_in = np.asarray(inputs["w_in"][0], dtype=f)
    o = np.cumsum([0, 512, 512, 64, 64, 256, 32, 8, 2048])
    c_up, c_q, c_k, c_v, c_iq, c_ik, c_iw, c_g = [slice(o[j], o[j + 1]) for j in range(8)]
    w_tok = np.ascontiguousarray(np.concatenate([w_in[:, c_up], w_in[:, c_g], w_in[:, c_v], w_in[:, c_iw]], axis=1))
    w_feat = np.ascontiguousarray(np.concatenate([w_in[:, c_q], w_in[:, c_k], w_in[:, c_iq]] + [w_in[:, c_ik]] * 3, axis=1))

    def rep(v):
        v = np.asarray(v, dtype=f).reshape(1, -1)
        return np.ascontiguousarray(np.broadcast_to(v, (128, v.shape[1])))

    def col(v):
        return np.ascontiguousarray(np.asarray(v, dtype=f).reshape(KC, 128).T)

    band = np.zeros((128, 12, 128), dtype=f)
    tp = np.arange(128)[:, None]
    tq = np.arange(128)[None, :]
    for g, w in enumerate((2, 4, 8, 16)):
        cur = ((tp <= tq) & (tp > tq - w)).astype(f) / w - (tp == tq).astype(f)
        prev = ((tp - 128) > (tq - w)).astype(f) / w
        cnt = np.minimum(w, tq + 1).astype(f)
        cur0 = ((tp <= tq) & (tp > tq - w)).astype(f) / cnt - (tp == tq).astype(f)
        band[:, g], band[:, 4 + g], band[:, 8 + g] = cur, prev, cur0
    cbias = np.where(tq <= tp, 0.0, -1e30).astype(f)
    pow2 = np.broadcast_to((2.0 ** -np.arange(NBIS + 1)).astype(f)[None, :], (128, NBIS + 1))
    common = {
        "w_in_tok": w_tok, "w_in_feat": w_feat,
        "ln_in_g": rep(inputs["ln_in_g"]), "ln_in_b": rep(inputs["ln_in_b"]),
        "ln1_g": rep(inputs["ln1_g"][0]), "ln1_b": rep(inputs["ln1_b"][0]),
        "ln2_g": rep(inputs["ln2_g"][0]), "ln2_b": rep(inputs["ln2_b"][0]),
        "ln_in_gc": col(inputs["ln_in_g"]), "ln_in_bc": col(inputs["ln_in_b"]),
        "ln1_gc": col(inputs["ln1_g"][0]), "ln1_bc": col(inputs["ln1_b"][0]),
        "ident": np.eye(128, dtype=f).astype(bf),
        "i4": np.ascontiguousarray(np.tile(np.eye(128, dtype=f), (1, 4))).astype(bf),
        "band": np.ascontiguousarray(band.reshape(128, 12 * 128)).astype(bf),
        "cbias": np.ascontiguousarray(cbias),
        "pow2": np.ascontiguousarray(pow2),
        "b_gate": np.ascontiguousarray(np.asarray(inputs["b_gate"], dtype=f).reshape(1, 2048)),
        "pool_w": np.ascontiguousarray(np.asarray(inputs["pool_w"][0], dtype=f)),
        "pool_scale": np.ascontiguousarray(np.asarray(inputs["pool_scale"][0], dtype=f).reshape(4, 128).T),
        "w_proj_pool": np.ascontiguousarray(np.asarray(inputs["w_proj_pool"][0], dtype=f)),
        "w_proj_attn": np.ascontiguousarray(np.asarray(inputs["w_proj_attn"][0], dtype=f)),
        "w_out": np.ascontiguousarray(np.asarray(inputs["w_out"][0], dtype=f)),
        "w_gr": np.ascontiguousarray(np.concatenate([np.asarray(inputs["w_group"][0], dtype=f),
                                                     np.asarray(inputs["w_router"][0], dtype=f)], axis=1)),
        "b_gr": rep(np.concatenate([np.asarray(inputs["b_group"][0], dtype=f), np.asarray(inputs["b_router"][0], dtype=f)])),
        "w_gate": np.ascontiguousarray(np.asarray(inputs["w_gate"][0], dtype=f)),
        "w_up": np.ascontiguousarray(np.asarray(inputs["w_up"][0], dtype=f)),
        "w_down": np.ascontiguousarray(np.asarray(inputs["w_down"][0], dtype=f)),
    }
    maps = []
    for c in range(8):
        m = dict(common)
        m["x"] = np.ascontiguousarray(np.asarray(inputs["x"][c], dtype=f))
        maps.append(m)
    return maps


def kernel(**inputs):
    nc = build({})
    maps = host_inputs(inputs)
    res = run_bass_kernel_spmd(nc, maps, core_ids=list(range(8)))
    return np.stack([r["out"] for r in res.results], axis=0).astype(np.float32)
```

```python
import numpy as np
import ml_dtypes
from contextlib import ExitStack
import concourse.bass as bass
import concourse.mybir as mybir
from concourse.bass_utils import run_bass_kernel_spmd

F32 = mybir.dt.float32
BF16 = mybir.dt.bfloat16
AF = mybir.ActivationFunctionType
ALU = mybir.AluOpType
AX = mybir.AxisListType

T = 4096
NT = 32
D = 1024
KC = 8
ALPHA = 2.0 ** 0.25
EPS = 1e-5
SM_SCALE = 0.125
NSEL = 256
NBIS = 20
CTOK = 512 + 2048 + 64 + 8
CFEAT = 512 + 64 + 256 + 96
NEG = -30000.0
ACT_SPLIT_MIN = 1536


class Tok:
    __slots__ = ("sem", "val", "need", "dma")


class Buf:
    def __init__(self):
        self.w = {}
        self.r = {}


class Prog:
    def __init__(self, nc, es):
        self.nc = nc
        self.es = es
        self.L = {e: [] for e in ("pe", "act", "dve", "pool", "sp")}
        self.esem = {e: es.enter_context(nc.semaphore("s_" + e)) for e in self.L}
        self.dsem = {}
        self.dcnt = {}
        self.nops = 0

    def _deps(self, reads, writes, extra):
        deps = {}
        for b in reads:
            for k, t in b.w.items():
                deps[(k, t.val if t.dma else id(t))] = t
        for b in writes:
            for k, t in b.w.items():
                deps[(k, t.val if t.dma else id(t))] = t
            for k, t in b.r.items():
                deps[(k, t.val if t.dma else id(t))] = t
        for t in extra:
            if t is not None:
                deps[(id(t.sem), id(t))] = t
        return list(deps.values())

    def _post(self, t, reads, writes, acc):
        k = id(t.sem)
        for b in reads:
            b.r[k] = t
        for b in writes:
            if not acc:
                b.w = {}
            b.r = {}
            b.w[k] = t

    def op(self, eng, fn, reads=(), writes=(), acc=False, extra=()):
        L = self.L[eng]
        for d in self._deps(reads, writes, extra):
            if eng == "pe" and d.sem is self.esem["pe"]:
                continue
            d.need = True
            L.append(d)
        t = Tok()
        t.sem = self.esem[eng]
        t.val = None
        t.need = False
        t.dma = False
        L.append((fn, t))
        self._post(t, reads, writes, acc)
        self.nops += 1
        return t

    def dma(self, eng, out, in_, reads=(), writes=(), acc=False, extra=(), stream="d"):
        if stream not in self.dsem:
            self.dsem[stream] = self.es.enter_context(self.nc.semaphore("d_" + stream))
            self.dcnt[stream] = 0
        L = self.L[eng]
        for d in self._deps(reads, writes, extra):
            d.need = True
            L.append(d)
        self.dcnt[stream] += 16
        t = Tok()
        t.sem = self.dsem[stream]
        t.val = self.dcnt[stream]
        t.need = True
        t.dma = True
        L.append((lambda e: e.dma_start(out=out, in_=in_), t))
        self._post(t, reads, writes, acc)
        return t

    def finalize(self, final_waits):
        for e, L in self.L.items():
            c = 0
            for it in L:
                if isinstance(it, tuple):
                    fn, t = it
                    if (not t.dma) and t.need:
                        c += 1
                        t.val = c
        nc = self.nc

        def replay(e, eng):
            waited = {}
            for it in self.L[e]:
                if isinstance(it, Tok):
                    k = id(it.sem)
                    if waited.get(k, 0) >= it.val:
                        continue
                    eng.wait_ge(it.sem, it.val)
                    waited[k] = it.val
                else:
                    fn, t = it
                    ins = fn(eng)
                    if t.dma:
                        ins.then_inc(t.sem, 16)
                    elif t.need:
                        ins.then_inc(t.sem, 1)
            if e == "sp":
                for t in final_waits:
                    eng.wait_ge(t.sem, t.val)

        with nc.Block() as block:
            @block.tensor
            def _(eng):
                replay("pe", eng)

            @block.scalar
            def _(eng):
                replay("act", eng)

            @block.vector
            def _(eng):
                replay("dve", eng)

            @block.gpsimd
            def _(eng):
                replay("pool", eng)

            @block.sync
            def _(eng):
                replay("sp", eng)


class PsumPool:
    def __init__(self, banks):
        self.banks = banks
        self.i = 0

    def get(self):
        b = self.banks[self.i % len(self.banks)]
        self.i += 1
        return b


def build(cfg):
    ntiles = cfg.get("ntiles", NT)
    dbg = cfg.get("dbg", ())
    do_moe = cfg.get("moe", True)
    nexp = cfg.get("nexp", 32)
    nc = bass.Bass("TRN2", target_bir_lowering=False)
    es = ExitStack()
    with es:
        P = Prog(nc, es)

        def dram_in(name, shape, dt=F32):
            return nc.dram_tensor(name, list(shape), dt, kind="ExternalInput").ap()

        def dram_out(name, shape, dt=F32):
            return nc.dram_tensor(name, list(shape), dt, kind="ExternalOutput").ap()

        def sb(name, shape, dt):
            return es.enter_context(nc.sbuf_tensor("sb_" + name, list(shape), dt))

        B_ = {}

        def bufof(name):
            if name not in B_:
                B_[name] = Buf()
            return B_[name]

        x_d = dram_in("x", [T, D])
        wtok_d = dram_in("w_in_tok", [D, CTOK]).rearrange("(kc p) c -> p kc c", p=128)
        wfeat_d = dram_in("w_in_feat", [D, CFEAT]).rearrange("(kc p) c -> p kc c", p=128)
        lng_d = dram_in("ln_in_g", [128, D])
        lnb_d = dram_in("ln_in_b", [128, D])
        ln1g_d = dram_in("ln1_g", [128, D])
        ln1b_d = dram_in("ln1_b", [128, D])
        ln2g_d = dram_in("ln2_g", [128, D])
        ln2b_d = dram_in("ln2_b", [128, D])
        lngc_d = dram_in("ln_in_gc", [128, KC])
        lnbc_d = dram_in("ln_in_bc", [128, KC])
        ln1gc_d = dram_in("ln1_gc", [128, KC])
        ln1bc_d = dram_in("ln1_bc", [128, KC])
        ident_d = dram_in("ident", [128, 128], BF16)
        i4_d = dram_in("i4", [128, 512], BF16)
        band_d = dram_in("band", [128, 12 * 128], BF16)
        cbias_d = dram_in("cbias", [128, 128])
        pow2_d = dram_in("pow2", [128, NBIS + 1])
        bgate_d = dram_in("b_gate", [1, 2048])
        poolw_d = dram_in("pool_w", [4, 128, 128]).rearrange("g d e -> d g e")
        pscale_d = dram_in("pool_scale", [128, 4])
        wpp_d = dram_in("w_proj_pool", [512, D]).rearrange("(g p) c -> p g c", p=128)
        wpa_d = dram_in("w_proj_attn", [512, D]).rearrange("(g p) c -> p g c", p=128)
        wout_d = dram_in("w_out", [D, D]).rearrange("(g p) c -> p g c", p=128)
        wr_d = dram_in("w_gr", [D, 36]).rearrange("(g p) c -> p g c", p=128)
        br_d = dram_in("b_gr", [128, 36])
        wg_d = dram_in("w_gate", [32, D, 256]).rearrange("e (kc p) f -> e p kc f", p=128)
        wu_d = dram_in("w_up", [32, D, 256]).rearrange("e (kc p) f -> e p kc f", p=128)
        wd_d = dram_in("w_down", [32, 256, D]).rearrange("e (fc p) d -> e p fc d", p=128)
        out_d = dram_out("out", [T, D])
        h2_d = nc.dram_tensor("h2_scr", [T, D], F32, kind="Internal").ap()
        h2T_d = nc.dram_tensor("h2T_scr", [128, KC, T], BF16, kind="Internal").ap()
        hscr_d = nc.dram_tensor("h_scr", [T, D], F32, kind="Internal").ap()
        dbg_d = {}
        for name, shape in dbg:
            dbg_d[name] = dram_out("dbg_" + name, shape)

        A_WTOK, A_WFEAT, A_WOUT = 0, KC * CTOK, KC * CTOK + KC * CFEAT
        A_WPP = A_WOUT + KC * D
        A_WPA = A_WPP + 4 * D
        A_SC = A_WPA + 4 * D
        A_MB = A_SC + 2 * T
        A_END = A_MB + T
        arena = sb("arena", [128, A_END], BF16)
        wtok = arena[:, A_WTOK:A_WFEAT].rearrange("p (k c) -> p k c", k=KC)
        wfeat = arena[:, A_WFEAT:A_WOUT].rearrange("p (k c) -> p k c", k=KC)
        wout = arena[:, A_WOUT:A_WPP].rearrange("p (k c) -> p k c", k=KC)
        wpp = arena[:, A_WPP:A_WPA].rearrange("p (k c) -> p k c", k=4)
        wpa = arena[:, A_WPA:A_SC].rearrange("p (k c) -> p k c", k=4)
        scores = arena[:, A_SC:A_MB].bitcast(F32)
        mb = arena[:, A_MB:A_END]

        ident = sb("ident", [128, 128], BF16)
        i4 = sb("i4", [128, 512], BF16)
        band = sb("band", [128, 12, 128], BF16)
        cbias = sb("cbias", [128, 128], F32)
        pow2 = sb("pow2", [128, NBIS + 1], F32)
        lncol = sb("lncol", [128, 4, KC], F32)
        epst = sb("epst", [128, 1], F32)
        eps2t = sb("eps2t", [128, 1], F32)
        ones1 = sb("ones1", [1, 128], BF16)
        bgate = sb("bgate", [1, 2048], BF16)
        poolw = sb("poolw", [128, 4, 128], BF16)
        pscale = sb("pscale", [128, 4], F32)
        wr = sb("wr", [128, KC, 36], BF16)
        brt = sb("brt", [128, 36], F32)
        lng = sb("lng", [128, D], F32)
        lnb = sb("lnb", [128, D], F32)
        ln1g = sb("ln1g", [128, D], F32)
        ln1b = sb("ln1b", [128, D], F32)
        htok = [sb("htok%d" % i, [128, D], F32) for i in range(2)]
        hbf = sb("hbf", [128, D], BF16)
        hT = sb("hT", [128, KC, 128], BF16)
        stats = sb("stats", [128, 2, 6], F32)
        mv = sb("mv", [128, 2], F32)
        rstd = sb("rstd", [128, 1], F32)
        mv8 = sb("mv8", [128, 8, 2], F32)
        rs8 = sb("rs8", [128, 16], F32)
        nmr = sb("nmr", [128, 1], F32)
        upool = [sb("upool%d" % i, [128, 512], BF16) for i in range(2)]
        gates = [sb("gates%d" % i, [128, 2048], BF16) for i in range(2)]
        kT = sb("kT", [64, T], BF16)
        ikT4 = sb("ikT4", [128, T], BF16)
        vaug = sb("vaug", [128, NT, 65], BF16)
        qT = [sb("qT%d" % i, [64, 8 * 128], BF16) for i in range(2)]
        iqT = sb("iqT", [128, 3, 128], BF16)
        iwt = sb("iwt", [128, 8], F32)
        NRL = 4
        rl = [sb("rl%d" % i, [128, 512], F32) for i in range(NRL)]
        junk1 = sb("junk1", [128, 1], BF16)
        junk = junk1[:, 0:1].to_broadcast([128, T])
        junk2t = sb("junk2", [128, 1], BF16)
        junk2 = junk2t[:, 0:1].to_broadcast([128, T])
        sga = sb("sga", [128, 1], F32)
        mabs = sb("mabs", [128, 1], F32)
        wk = sb("wk", [128, NBIS + 1], F32)
        tcur = sb("tcur", [128, 1], F32)
        cnt = sb("cnt", [128, 1], F32)
        sgn = sb("sgn", [128, 1], F32)
        thr = sb("thr", [128, 1], F32)
        PT0 = sb("PT0", [128, 1024], BF16)
        PT = [PT0, PT0]
        rden = sb("rden", [128, 8], F32)
        hbf2 = sb("hbf2", [128, D], BF16)
        hT2 = sb("hT2", [128, KC, 128], BF16)
        ao = hbf2[:, 0:512]
        aoT = hT2[:, 0:4, :]
        deltaT = sb("deltaT", [128, 4, 128], BF16)
        poT = [sb("poT%d" % i, [128, 4, 128], BF16) for i in range(2)]
        mg = hbf2
        mgT = hT2
        h2bf = hbf2
        h2T = hT2
        rt = sb("rt", [128, 36], F32)
        rsm = sb("rsm", [128, 16], F32)
        m8 = sb("m8", [128, 8], F32)
        g1t = sb("g1t", [128, 32], F32)
        g2t = sb("g2t", [128, 32], F32)
        gtall = sb("gtall", [128, NT, 32], F32)

        psum = [es.enter_context(nc.psum_tensor("ps%d" % i, [128, 512], F32)) for i in range(8)]
        pbuf = [Buf() for _ in range(8)]
        gen = PsumPool(list(range(0, 6)))

        BW = bufof("w")
        P.dma("sp", ident[:], ident_d, writes=[BW], acc=True, stream="c")
        P.dma("sp", i4[:], i4_d, writes=[BW], acc=True, stream="c")
        P.dma("sp", band[:].rearrange("p a t -> p (a t)"), band_d, writes=[BW], acc=True, stream="c")
        P.dma("sp", cbias[:], cbias_d, writes=[BW], acc=True, stream="c")
        P.dma("sp", pow2[:], pow2_d, writes=[BW], acc=True, stream="c")
        P.dma("sp", pscale[:], pscale_d, writes=[BW], acc=True, stream="c")
        P.dma("sp", brt[:], br_d, writes=[BW], acc=True, stream="c")
        for j, dd in enumerate((lngc_d, lnbc_d, ln1gc_d, ln1bc_d)):
            P.dma("sp", lncol[:, j, :], dd, writes=[BW], acc=True, stream="c")
        P.dma("sp", lng[:], lng_d, writes=[bufof("lng")], stream="lg")
        P.dma("sp", lnb[:], lnb_d, writes=[bufof("lnb")], stream="lb")
        P.dma("sp", ln1g[:], ln1g_d, writes=[BW], acc=True, stream="c")
        P.dma("sp", ln1b[:], ln1b_d, writes=[BW], acc=True, stream="c")
        P.op("dve", lambda eng: eng.memset(epst[:], EPS), writes=[BW], acc=True)
        P.op("dve", lambda eng: eng.memset(eps2t[:], EPS / (ALPHA * ALPHA)), writes=[BW], acc=True)
        P.op("dve", lambda eng: eng.memset(ones1[:], 1.0), writes=[BW], acc=True)
        P.op("pool", lambda eng: eng.memset(vaug[:].rearrange("p a c -> p (a c)"), 1.0), writes=[bufof("vaug")])

        cast_n = [0]

        last_cast = {}

        def stage_cast(src, dst, rows, cols):
            for c0 in range(0, cols, 1024):
                c1 = min(cols, c0 + 1024)
                n = cast_n[0]
                cast_n[0] += 1
                j = n % 4
                s_ = scores[:, j * 1024:(j + 1) * 1024]
                sbf = bufof("stgS%d" % j)
                P.dma("sp", s_[0:rows, 0:c1 - c0], src[:, c0:c1], writes=[sbf], stream="w%d" % j)
                e = ("act", "dve", "pool", "act")[n % 4]
                if e == "act":
                    t = P.op("act", lambda eng, d=dst, s_=s_, c0=c0, c1=c1: eng.copy(out=d[:, c0:c1], in_=s_[0:rows, 0:c1 - c0]),
                             reads=[sbf], writes=[BW], acc=True)
                else:
                    t = P.op(e, lambda eng, d=dst, s_=s_, c0=c0, c1=c1: eng.tensor_copy(out=d[:, c0:c1], in_=s_[0:rows, 0:c1 - c0]),
                             reads=[sbf], writes=[BW], acc=True)
                last_cast[e] = t

        for kc in range(KC):
            stage_cast(wtok_d[:, kc, :], wtok[:, kc, :], 128, CTOK)
            stage_cast(wfeat_d[:, kc, :], wfeat[:, kc, :], 128, CFEAT)
        for g in range(4):
            stage_cast(wpp_d[:, g, :], wpp[:, g, :], 128, D)
            stage_cast(wpa_d[:, g, :], wpa[:, g, :], 128, D)
            stage_cast(poolw_d[:, g, :], poolw[:, g, :], 128, 128)
        for kc in range(KC):
            stage_cast(wout_d[:, kc, :], wout[:, kc, :], 128, D)
            stage_cast(wr_d[:, kc, :], wr[:, kc, :], 128, 36)
        stage_cast(bgate_d, bgate[:], 1, 2048)
        for t in last_cast.values():
            bufof("scores").r[id(t.sem)] = t

        def layer_norm(src, dst, gt, bt, srcbuf, dstbuf, gbufs, bf=None, bfbuf=None):
            for c in range(2):
                P.op("dve", lambda eng, c=c: eng.bn_stats(out=stats[:, c, :], in_=src[:, c * 512:(c + 1) * 512]),
                     reads=[srcbuf], writes=[bufof("stats")], acc=(c > 0))
            P.op("dve", lambda eng: eng.bn_aggr(out=mv[:], in_=stats[:].rearrange("p a s -> p (a s)")),
                 reads=[bufof("stats")], writes=[bufof("mv")])
            P.op("act", lambda eng: eng.activation(out=rstd[:], in_=mv[:, 1:2], func=AF.Sqrt, bias=epst[:], scale=1.0),
                 reads=[bufof("mv"), BW], writes=[bufof("rstd")])
            P.op("dve", lambda eng: eng.reciprocal(out=rstd[:], in_=rstd[:]), writes=[bufof("rstd")])
            P.op("dve", lambda eng: eng.scalar_tensor_tensor(out=nmr[:], in0=mv[:, 0:1], scalar=-1.0, in1=rstd[:],
                                                             op0=ALU.mult, op1=ALU.mult),
                 reads=[bufof("mv"), bufof("rstd")], writes=[bufof("nmr")])
            if bf is not None:
                P.op("act", lambda eng: eng.activation(out=bf[:], in_=src[:], func=AF.Identity, bias=nmr[:], scale=rstd[:]),
                     reads=[srcbuf, bufof("rstd"), bufof("nmr")], writes=[bfbuf])
            P.op("act", lambda eng: eng.activation(out=dst[:], in_=src[:], func=AF.Identity, bias=nmr[:], scale=rstd[:]),
                 reads=[srcbuf, bufof("rstd"), bufof("nmr")], writes=[dstbuf])
            P.op("pool", lambda eng: eng.tensor_mul(out=dst[:], in0=dst[:], in1=gt[:]), reads=gbufs, writes=[dstbuf])
            P.op("pool", lambda eng: eng.tensor_add(out=dst[:], in0=dst[:], in1=bt[:]), reads=gbufs, writes=[dstbuf])

        def transpose_to(src_bf, nblk, dst_flat, srcbuf, dstbuf, gb=None):
            pb = gen.get()
            pT = psum[pb][:].bitcast(BF16)
            for kc in range(nblk):
                P.op("pe", lambda eng, kc=kc, pT=pT: eng.transpose(out=pT[:, kc * 128:(kc + 1) * 128],
                                                                   in_=src_bf[:, kc * 128:(kc + 1) * 128], identity=ident[:]),
                     reads=[srcbuf, BW], writes=[pbuf[pb]], acc=(kc > 0))
            if gb is None:
                P.op("dve", lambda eng, pT=pT: eng.tensor_copy(out=dst_flat, in_=pT[:, 0:nblk * 128]),
                     reads=[pbuf[pb]], writes=[dstbuf])
            else:
                for kc in range(nblk):
                    P.op("dve", lambda eng, pT=pT, kc=kc: eng.tensor_scalar(
                        out=dst_flat[:, kc * 128:(kc + 1) * 128], in0=pT[:, kc * 128:(kc + 1) * 128],
                        scalar1=lncol[:, gb, kc:kc + 1], scalar2=lncol[:, gb + 1, kc:kc + 1], op0=ALU.mult, op1=ALU.add),
                        reads=[pbuf[pb], BW], writes=[dstbuf], acc=(kc > 0))

        final = []
        HB, HT = bufof("hbf"), bufof("hT")
        SC, MB = bufof("scores"), bufof("mb")

        def dbg_store(name, i, src, srcbuf, width):
            if name in dbg_d:
                final.append(P.dma("pool", dbg_d[name][i * 128:(i + 1) * 128, 0:width], src, reads=[srcbuf], stream="o"))

        def advance(g, n=1):
            if g is None:
                return
            for _ in range(n):
                try:
                    next(g)
                except StopIteration:
                    return

        def drain(g):
            if g is None:
                return
            for _ in g:
                pass

        def stage_X(i, g3=None, g2=None):
            H = bufof("htok0")
            P.dma("sp", htok[0][:], x_d[i * 128:(i + 1) * 128, :], writes=[H], stream="x0")
            layer_norm(htok[0], htok[0], lng, lnb, H, H, [bufof("lng"), bufof("lnb")], bf=hbf, bfbuf=HB)
            P.dma("pool", hscr_d[i * 128:(i + 1) * 128, :], htok[0][:], reads=[H], writes=[bufof("hscr%d" % (i % 4))], stream="hs%d" % (i % 4))
            transpose_to(hbf, KC, hT[:].rearrange("p k t -> p (k t)"), HB, HT, gb=0)
            pb = gen.get()
            for kc in range(KC):
                P.op("pe", lambda eng, kc=kc, pb=pb: eng.matmul(psum[pb][:, 0:72], lhsT=hT[:, kc, :], rhs=wtok[:, kc, 2560:2632],
                                                                start=(kc == 0), stop=(kc == KC - 1)),
                     reads=[HT, BW], writes=[pbuf[pb]], acc=(kc > 0))
            P.op("dve", lambda eng, pb=pb: eng.tensor_copy(out=vaug[:, i, 0:64], in_=psum[pb][:, 0:64]),
                 reads=[pbuf[pb]], writes=[bufof("vaug")], acc=True)
            P.op("dve", lambda eng, pb=pb: eng.tensor_copy(out=iwt[:], in_=psum[pb][:, 64:72]),
                 reads=[pbuf[pb]], writes=[bufof("iwt")])
            pb = gen.get()
            fm = ((512, 64), (576, 96), (672, 96), (768, 64))
            for j, (c0, m) in enumerate(fm):
                for kc in range(KC):
                    P.op("pe", lambda eng, kc=kc, pb=pb, j=j, c0=c0, m=m: eng.matmul(
                        psum[pb][0:m, j * 128:(j + 1) * 128], lhsT=wfeat[:, kc, c0:c0 + m], rhs=hT[:, kc, :],
                        start=(kc == 0), stop=(kc == KC - 1)),
                        reads=[HT, BW], writes=[pbuf[pb]], acc=(kc > 0 or j > 0))
            P.op("dve", lambda eng, pb=pb: eng.tensor_copy(out=kT[:, i * 128:(i + 1) * 128], in_=psum[pb][0:64, 0:128]),
                 reads=[pbuf[pb]], writes=[bufof("kT")], acc=True)
            P.op("dve", lambda eng, pb=pb: eng.tensor_copy(out=iqT[0:96, 0:2, :].rearrange("p a t -> p (a t)"), in_=psum[pb][0:96, 128:384]),
                 reads=[pbuf[pb]], writes=[bufof("iqT")])
            P.op("dve", lambda eng, pb=pb: eng.tensor_copy(out=iqT[0:64, 2, :], in_=psum[pb][0:64, 384:512]),
                 reads=[pbuf[pb]], writes=[bufof("iqT")], acc=True)
            pb = gen.get()
            for kc in range(KC):
                P.op("pe", lambda eng, kc=kc, pb=pb: eng.matmul(psum[pb][0:96, 0:128], lhsT=wfeat[:, kc, 832:928], rhs=hT[:, kc, :],
                                                                start=(kc == 0), stop=(kc == KC - 1)),
                     reads=[HT, BW], writes=[pbuf[pb]], acc=(kc > 0))
            P.op("act", lambda eng, pb=pb: eng.copy(out=ikT4[0:96, i * 128:(i + 1) * 128], in_=psum[pb][0:96, 0:128]),
                 reads=[pbuf[pb]], writes=[bufof("ikT4")], acc=True)
            S = 128 * (i + 1)
            nch = (S + 511) // 512
            nrl = 0
            n_it = 8 * nch
            cx = i // 3
            every2 = max(1, n_it // (cx + 1))
            advance(g3, 1)
            for c in range(nch):
                w = min(512, S - c * 512)
                for h in range(8):
                    if nrl % 3 == 2:
                        advance(g3, 1)
                    if cx > 0 and nrl % every2 == every2 - 1:
                        advance(g2, 1)
                    pb = gen.get()
                    hp, hc = h % 3, h // 3
                    P.op("pe", lambda eng, pb=pb, hp=hp, hc=hc, c=c, w=w: eng.matmul(
                        psum[pb][:, 0:w], lhsT=iqT[hp * 32:(hp + 1) * 32, hc, :], rhs=ikT4[hp * 32:(hp + 1) * 32, c * 512:c * 512 + w],
                        start=True, stop=True),
                        reads=[bufof("iqT"), bufof("ikT4")], writes=[pbuf[pb]])
                    r = nrl % NRL
                    nrl += 1
                    RL = bufof("rl%d" % r)
                    P.op("act", lambda eng, pb=pb, r=r, w=w: eng.activation(out=rl[r][:, 0:w], in_=psum[pb][:, 0:w], func=AF.Relu),
                         reads=[pbuf[pb]], writes=[RL])
                    if h == 0:
                        P.op("dve", lambda eng, r=r, c=c, w=w: eng.tensor_scalar(
                            out=scores[:, c * 512:c * 512 + w], in0=rl[r][:, 0:w], scalar1=iwt[:, 0:1], scalar2=None, op0=ALU.mult),
                            reads=[RL, bufof("iwt")], writes=[SC], acc=(c > 0))
                    else:
                        P.op("dve", lambda eng, r=r, c=c, w=w, h=h: eng.scalar_tensor_tensor(
                            out=scores[:, c * 512:c * 512 + w], in0=rl[r][:, 0:w], scalar=iwt[:, h:h + 1],
                            in1=scores[:, c * 512:c * 512 + w], op0=ALU.mult, op1=ALU.add),
                            reads=[RL, bufof("iwt")], writes=[SC], acc=True)

        def stage_Y(i, g3=None, g2=None, n2=2):
            S = 128 * (i + 1)
            P.op("dve", lambda eng: eng.tensor_reduce(out=mabs[:], in_=scores[:, 0:S], axis=AX.X, op=ALU.max,
                                                      apply_absolute_value=True),
                 reads=[SC], writes=[bufof("mabs")])
            P.op("dve", lambda eng: eng.tensor_tensor(out=scores[:, i * 128:(i + 1) * 128], in0=scores[:, i * 128:(i + 1) * 128],
                                                      in1=cbias[:], op=ALU.add),
                 reads=[BW], writes=[SC])
            P.op("dve", lambda eng: eng.tensor_scalar(out=mabs[:], in0=mabs[:], scalar1=1.001, scalar2=1e-20, op0=ALU.mult, op1=ALU.add),
                 writes=[bufof("mabs")])
            P.op("dve", lambda eng: eng.tensor_scalar(out=wk[:], in0=pow2[:], scalar1=mabs[:, 0:1], scalar2=None, op0=ALU.mult),
                 reads=[bufof("mabs"), BW], writes=[bufof("wk")])
            P.op("dve", lambda eng: eng.memset(tcur[:], 0.0), writes=[bufof("tcur")])
            JK = bufof("junk")
            split = (S >= ACT_SPLIT_MIN)
            Sd = (S // 2) if split else S
            na = S - Sd
            for k in range(NBIS if S > NSEL else 0):
                P.op("dve", lambda eng: eng.tensor_scalar(out=junk[:, 0:Sd], in0=scores[:, 0:Sd], scalar1=tcur[:, 0:1], scalar2=0.0,
                                                          op0=ALU.is_ge, op1=ALU.add, accum_out=cnt[:]),
                     reads=[SC, bufof("tcur")], writes=[JK, bufof("cnt")])
                if split:
                    P.op("act", lambda eng: eng.activation(out=junk2[:, 0:na], in_=scores[:, Sd:S], func=AF.Sign, bias=tcur[:, 0:1],
                                                           scale=-1.0, accum_out=sga[:]),
                         reads=[SC, bufof("tcur")], writes=[bufof("junk2"), bufof("sga")])
                    P.op("dve", lambda eng: eng.scalar_tensor_tensor(out=cnt[:], in0=cnt[:], scalar=2.0, in1=sga[:],
                                                                     op0=ALU.mult, op1=ALU.subtract),
                         reads=[bufof("sga")], writes=[bufof("cnt")])
                    P.op("dve", lambda eng: eng.tensor_scalar(out=sgn[:], in0=cnt[:], scalar1=float(2 * NSEL - na), scalar2=-0.5,
                                                              op0=ALU.is_ge, op1=ALU.add),
                         reads=[bufof("cnt")], writes=[bufof("sgn")])
                else:
                    P.op("dve", lambda eng: eng.tensor_scalar(out=sgn[:], in0=cnt[:], scalar1=float(NSEL), scalar2=-0.5,
                                                              op0=ALU.is_ge, op1=ALU.add),
                         reads=[bufof("cnt")], writes=[bufof("sgn")])
                P.op("dve", lambda eng, k=k: eng.scalar_tensor_tensor(out=tcur[:], in0=sgn[:], scalar=wk[:, k:k + 1], in1=tcur[:],
                                                                      op0=ALU.mult, op1=ALU.add),
                     reads=[bufof("sgn"), bufof("wk")], writes=[bufof("tcur")])
                advance(g3, 1)
                advance(g2, n2)
            drain(g2)
            if S > NSEL:
                P.op("dve", lambda eng: eng.scalar_tensor_tensor(out=thr[:], in0=wk[:, NBIS:NBIS + 1], scalar=-1.0, in1=tcur[:],
                                                                 op0=ALU.mult, op1=ALU.add),
                     reads=[bufof("wk"), bufof("tcur")], writes=[bufof("thr")])
            else:
                P.op("dve", lambda eng: eng.tensor_scalar(out=thr[:], in0=wk[:, 0:1], scalar1=-1.0, scalar2=None, op0=ALU.mult),
                     reads=[bufof("wk")], writes=[bufof("thr")])
            P.op("dve", lambda eng: eng.tensor_scalar(out=mb[:, 0:S], in0=scores[:, 0:S], scalar1=thr[:, 0:1], scalar2=NEG,
                                                      op0=ALU.is_lt, op1=ALU.mult),
                 reads=[SC, bufof("thr")], writes=[MB])
            dbg_store("thr", i, thr[:], bufof("thr"), 1)
            drain(g3)
            drain(g2)

        def stage_W(i):
            ub = i % 2
            UP, UPP = bufof("upool%d" % ub), bufof("upool%d" % (1 - ub))
            GT, QT, POT = bufof("gates%d" % ub), bufof("qT%d" % ub), bufof("poT%d" % ub)
            pb = gen.get()
            for kc in range(KC):
                P.op("pe", lambda eng, kc=kc, pb=pb: eng.matmul(psum[pb][:], lhsT=hT[:, kc, :], rhs=wtok[:, kc, 0:512],
                                                                start=(kc == 0), stop=(kc == KC - 1)),
                     reads=[HT, BW], writes=[pbuf[pb]], acc=(kc > 0))
            P.op("act", lambda eng, pb=pb: eng.copy(out=upool[ub][:], in_=psum[pb][:]), reads=[pbuf[pb]], writes=[UP])
            for j in range(4):
                pb = gen.get()
                for kc in range(KC):
                    P.op("pe", lambda eng, kc=kc, pb=pb, j=j: eng.matmul(psum[pb][:], lhsT=hT[:, kc, :],
                                                                         rhs=wtok[:, kc, 512 + j * 512:1024 + j * 512],
                                                                         start=(kc == 0), stop=False),
                         reads=[HT, BW], writes=[pbuf[pb]], acc=(kc > 0))
                P.op("pe", lambda eng, pb=pb, j=j: eng.matmul(psum[pb][:], lhsT=ones1[0:1, :], rhs=bgate[0:1, j * 512:(j + 1) * 512],
                                                              start=False, stop=True),
                     reads=[BW], writes=[pbuf[pb]], acc=True)
                P.op("act", lambda eng, pb=pb, j=j: eng.activation(out=gates[ub][:, j * 512:(j + 1) * 512], in_=psum[pb][:], func=AF.Sigmoid),
                     reads=[pbuf[pb]], writes=[GT], acc=(j > 0))
            for half in range(2):
                pb = gen.get()
                for hh in range(4):
                    h = half * 4 + hh
                    for kc in range(KC):
                        P.op("pe", lambda eng, kc=kc, pb=pb, hh=hh, h=h: eng.matmul(
                            psum[pb][0:64, hh * 128:(hh + 1) * 128], lhsT=wfeat[:, kc, h * 64:(h + 1) * 64], rhs=hT[:, kc, :],
                            start=(kc == 0), stop=(kc == KC - 1)),
                            reads=[HT, BW], writes=[pbuf[pb]], acc=(kc > 0 or hh > 0))
                P.op("act", lambda eng, pb=pb, half=half: eng.copy(out=qT[ub][:, half * 512:(half + 1) * 512], in_=psum[pb][0:64, :]),
                     reads=[pbuf[pb]], writes=[QT], acc=(half > 0))
            pb = gen.get()
            for g in range(4):
                P.op("pe", lambda eng, pb=pb, g=g: eng.matmul(
                    psum[pb][:, g * 128:(g + 1) * 128], lhsT=upool[ub][:, g * 128:(g + 1) * 128],
                    rhs=band[:, (8 + g) if i == 0 else g, :], start=True, stop=(i == 0)),
                    reads=[UP, BW], writes=[pbuf[pb]], acc=(g > 0))
                if i > 0:
                    P.op("pe", lambda eng, pb=pb, g=g: eng.matmul(
                        psum[pb][:, g * 128:(g + 1) * 128], lhsT=upool[1 - ub][:, g * 128:(g + 1) * 128],
                        rhs=band[:, 4 + g, :], start=False, stop=True),
                        reads=[UPP, BW], writes=[pbuf[pb]], acc=True)
            P.op("act", lambda eng, pb=pb: eng.copy(out=deltaT[:].rearrange("p a t -> p (a t)"), in_=psum[pb][:]),
                 reads=[pbuf[pb]], writes=[bufof("deltaT")])
            pb = gen.get()
            for g in range(4):
                P.op("pe", lambda eng, pb=pb, g=g: eng.matmul(psum[pb][:, g * 128:(g + 1) * 128], lhsT=poolw[:, g, :],
                                                              rhs=deltaT[:, g, :], start=True, stop=True),
                     reads=[bufof("deltaT"), BW], writes=[pbuf[pb]], acc=(g > 0))
            for g in range(4):
                P.op("dve", lambda eng, pb=pb, g=g: eng.tensor_scalar(out=poT[ub][:, g, :], in0=psum[pb][:, g * 128:(g + 1) * 128],
                                                                      scalar1=pscale[:, g:g + 1], scalar2=None, op0=ALU.mult),
                     reads=[pbuf[pb], BW], writes=[POT], acc=(g > 0))

        def stage_S2(i):
            ub = i % 2
            QT = bufof("qT%d" % ub)
            for c in range(i + 1):
                pr = c % 2
                PTB = bufof("PT0")
                for half in range(2):
                    pb = gen.get()
                    P.op("pe", lambda eng, pb=pb, c=c, half=half: eng.matmul(
                        psum[pb][:], lhsT=kT[:, c * 128:(c + 1) * 128], rhs=qT[ub][:, half * 512:(half + 1) * 512], start=True, stop=False),
                        reads=[bufof("kT"), QT], writes=[pbuf[pb]])
                    P.op("pe", lambda eng, pb=pb, c=c: eng.matmul(
                        psum[pb][:], lhsT=mb[:, c * 128:(c + 1) * 128], rhs=i4[:], start=False, stop=True),
                        reads=[MB, BW], writes=[pbuf[pb]], acc=True)
                    P.op("act", lambda eng, pb=pb, pr=pr, half=half: eng.activation(
                        out=PT[pr][:, half * 512:(half + 1) * 512], in_=psum[pb][:], func=AF.Exp, scale=SM_SCALE),
                        reads=[pbuf[pb]], writes=[PTB], acc=(half > 0))
                for h in range(8):
                    ob = 6 + h // 4
                    P.op("pe", lambda eng, ob=ob, h=h, pr=pr, c=c: eng.matmul(
                        psum[ob][:, (h % 4) * 65:(h % 4) * 65 + 65], lhsT=PT[pr][:, h * 128:(h + 1) * 128], rhs=vaug[:, c, :],
                        start=(c == 0 and h % 4 == 0), stop=(c == i and h % 4 == 3)),
                        reads=[PTB, bufof("vaug")], writes=[pbuf[ob]], acc=not (c == 0 and h % 4 == 0))
                yield

        def stage_S3(i):
            ub = i % 2
            xb = 1
            H = bufof("htok1")
            GT, POT = bufof("gates%d" % ub), bufof("poT%d" % ub)
            HB, HT = bufof("hbf2"), bufof("hT2")
            P.dma("sp", htok[1][:], hscr_d[i * 128:(i + 1) * 128, :], reads=[bufof("hscr%d" % (i % 4))], writes=[H], stream="hl")
            for hb in range(2):
                ob = 6 + hb
                ov = psum[ob][:, 0:260].rearrange("p (h e) -> p h e", e=65)
                P.op("dve", lambda eng, ov=ov, hb=hb: eng.reciprocal(out=rden[:, hb * 4:(hb + 1) * 4], in_=ov[:, :, 64]),
                     reads=[pbuf[ob]], writes=[bufof("rden")], acc=(hb > 0))
                P.op("dve", lambda eng, ov=ov, hb=hb: eng.tensor_tensor(
                    out=ao[:, hb * 256:(hb + 1) * 256].rearrange("p (h e) -> p h e", e=64), in0=ov[:, :, 0:64],
                    in1=rden[:, hb * 4:(hb + 1) * 4].unsqueeze(2).to_broadcast([128, 4, 64]), op=ALU.mult),
                    reads=[pbuf[ob], bufof("rden")], writes=[HB], acc=(hb > 0))
            yield
            if "ao" in dbg_d:
                P.op("dve", lambda eng: eng.tensor_copy(out=rl[0][:], in_=ao), reads=[HB], writes=[bufof("rl0")])
                dbg_store("ao", i, rl[0][:], bufof("rl0"), 512)
            if "po" in dbg_d:
                P.op("dve", lambda eng: eng.tensor_copy(out=rl[1][:], in_=poT[ub][:].rearrange("p a t -> p (a t)")),
                     reads=[POT], writes=[bufof("rl1")])
                dbg_store("po", i, rl[1][:], bufof("rl1"), 512)
            transpose_to(ao, 4, aoT.rearrange("p a t -> p (a t)"), HB, HT)
            yield
            for n in range(2):
                pbp = gen.get()
                for g in range(4):
                    P.op("pe", lambda eng, pbp=pbp, g=g, n=n: eng.matmul(psum[pbp][:], lhsT=poT[ub][:, g, :], rhs=wpp[:, g, n * 512:(n + 1) * 512],
                                                                         start=(g == 0), stop=(g == 3)),
                         reads=[POT, BW], writes=[pbuf[pbp]], acc=(g > 0))
                pba = gen.get()
                for g in range(4):
                    P.op("pe", lambda eng, pba=pba, g=g, n=n: eng.matmul(psum[pba][:], lhsT=aoT[:, g, :], rhs=wpa[:, g, n * 512:(n + 1) * 512],
                                                                         start=(g == 0), stop=(g == 3)),
                         reads=[HT, BW], writes=[pbuf[pba]], acc=(g > 0))
                P.op("dve", lambda eng, pbp=pbp, n=n: eng.tensor_tensor(out=rl[0][:], in0=gates[ub][:, n * 512:(n + 1) * 512], in1=psum[pbp][:], op=ALU.mult),
                     reads=[GT, pbuf[pbp]], writes=[bufof("rl0")])
                P.op("dve", lambda eng, pba=pba, n=n: eng.tensor_tensor(out=rl[1][:], in0=gates[ub][:, 1024 + n * 512:1536 + n * 512], in1=psum[pba][:], op=ALU.mult),
                     reads=[GT, pbuf[pba]], writes=[bufof("rl1")])
                P.op("pool", lambda eng, n=n: eng.tensor_add(out=mg[:, n * 512:(n + 1) * 512], in0=rl[0][:], in1=rl[1][:]),
                     reads=[bufof("rl0"), bufof("rl1")], writes=[HB], acc=(n > 0))
                yield
            transpose_to(mg, KC, mgT[:].rearrange("p k t -> p (k t)"), HB, HT)
            yield
            for n in range(2):
                pb = gen.get()
                for kc in range(KC):
                    P.op("pe", lambda eng, pb=pb, kc=kc, n=n: eng.matmul(psum[pb][:], lhsT=mgT[:, kc, :], rhs=wout[:, kc, n * 512:(n + 1) * 512],
                                                                         start=(kc == 0), stop=(kc == KC - 1)),
                         reads=[HT, BW], writes=[pbuf[pb]], acc=(kc > 0))
                P.op("dve", lambda eng, pb=pb, n=n: eng.scalar_tensor_tensor(
                    out=htok[xb][:, n * 512:(n + 1) * 512], in0=htok[xb][:, n * 512:(n + 1) * 512], scalar=ALPHA, in1=psum[pb][:],
                    op0=ALU.mult, op1=ALU.add),
                    reads=[pbuf[pb]], writes=[H])
                yield
            h2 = htok[xb]
            layer_norm(h2, h2, ln1g, ln1b, H, H, [BW])
            dbg_store("h1", i, h2[:], H, D)
            if do_moe:
                P.dma("pool", h2_d[i * 128:(i + 1) * 128, :], h2[:], reads=[H], writes=[bufof("h2_d")], acc=True, stream="s1")
            P.op("act", lambda eng: eng.copy(out=h2bf[:], in_=h2[:]), reads=[H], writes=[HB])
            transpose_to(h2bf, KC, h2T[:].rearrange("p k t -> p (k t)"), HB, HT)
            yield
            if do_moe:
                P.dma("pool", h2T_d[:, :, i * 128:(i + 1) * 128], h2T[:], reads=[HT], writes=[bufof("h2T_d")], acc=True, stream="s2")
            pb = gen.get()
            for kc in range(KC):
                P.op("pe", lambda eng, pb=pb, kc=kc: eng.matmul(psum[pb][:, 0:36], lhsT=h2T[:, kc, :], rhs=wr[:, kc, :],
                                                                start=(kc == 0), stop=(kc == KC - 1)),
                     reads=[HT, BW], writes=[pbuf[pb]], acc=(kc > 0))
            RT, RS = bufof("rt"), bufof("rsm")
            P.op("dve", lambda eng, pb=pb: eng.tensor_tensor(out=rt[:], in0=psum[pb][:, 0:36], in1=brt[:], op=ALU.add),
                 reads=[pbuf[pb], BW], writes=[RT])
            P.op("dve", lambda eng: eng.tensor_reduce(out=rsm[:, 0:1], in_=rt[:, 0:4], axis=AX.X, op=ALU.max), reads=[RT], writes=[RS])
            P.op("dve", lambda eng: eng.tensor_scalar(out=rsm[:, 1:2], in0=rsm[:, 0:1], scalar1=-1.0, scalar2=None, op0=ALU.mult), writes=[RS])
            yield
            P.op("act", lambda eng: eng.activation(out=rsm[:, 12:16], in_=rt[:, 0:4], func=AF.Exp, bias=rsm[:, 1:2], scale=1.0,
                                                   accum_out=rsm[:, 2:3]), reads=[RT], writes=[RS])
            P.op("dve", lambda eng: eng.reciprocal(out=rsm[:, 3:4], in_=rsm[:, 2:3]), writes=[RS])
            P.op("dve", lambda eng: eng.tensor_scalar(out=rsm[:, 4:8], in0=rt[:, 0:4], scalar1=rsm[:, 0:1], scalar2=-1e30,
                                                      op0=ALU.is_lt, op1=ALU.mult), reads=[RT], writes=[RS])
            P.op("dve", lambda eng: eng.tensor_tensor(out=rt[:, 4:36].rearrange("p (g j) -> p g j", j=8),
                                                      in0=rt[:, 4:36].rearrange("p (g j) -> p g j", j=8),
                                                      in1=rsm[:, 4:8].unsqueeze(2).to_broadcast([128, 4, 8]), op=ALU.add),
                 reads=[RS], writes=[RT])
            P.op("dve", lambda eng: eng.max(out=m8[:], in_=rt[:, 4:36]), reads=[RT], writes=[bufof("m8")])
            P.op("dve", lambda eng: eng.tensor_tensor(out=rsm[:, 8:9], in0=m8[:, 1:2], in1=m8[:, 0:1], op=ALU.subtract),
                 reads=[bufof("m8")], writes=[RS])
            yield
            P.op("act", lambda eng: eng.activation(out=rsm[:, 9:10], in_=rsm[:, 8:9], func=AF.Exp), writes=[RS])
            P.op("dve", lambda eng: eng.tensor_scalar(out=rsm[:, 10:11], in0=rsm[:, 9:10], scalar1=1.0, scalar2=None, op0=ALU.add), writes=[RS])
            P.op("dve", lambda eng: eng.reciprocal(out=rsm[:, 10:11], in_=rsm[:, 10:11]), writes=[RS])
            P.op("dve", lambda eng: eng.scalar_tensor_tensor(out=rsm[:, 10:11], in0=rsm[:, 10:11], scalar=1.0 / ALPHA, in1=rsm[:, 3:4],
                                                             op0=ALU.mult, op1=ALU.mult), writes=[RS])
            P.op("dve", lambda eng: eng.tensor_tensor(out=rsm[:, 11:12], in0=rsm[:, 10:11], in1=rsm[:, 9:10], op=ALU.mult), writes=[RS])
            P.op("dve", lambda eng: eng.tensor_scalar(out=g1t[:], in0=rt[:, 4:36], scalar1=m8[:, 0:1], scalar2=rsm[:, 10:11],
                                                      op0=ALU.is_equal, op1=ALU.mult), reads=[RT, RS, bufof("m8")], writes=[bufof("g1t")])
            P.op("dve", lambda eng: eng.tensor_scalar(out=g2t[:], in0=rt[:, 4:36], scalar1=m8[:, 1:2], scalar2=rsm[:, 11:12],
                                                      op0=ALU.is_equal, op1=ALU.mult), reads=[RT, RS, bufof("m8")], writes=[bufof("g2t")])
            P.op("dve", lambda eng: eng.tensor_tensor(out=gtall[:, i, :], in0=g1t[:], in1=g2t[:], op=ALU.add),
                 reads=[bufof("g1t"), bufof("g2t")], writes=[bufof("gtall")], acc=True)
            dbg_store("gm", i, gtall[:, i, :], bufof("gtall"), 32)
            if not do_moe:
                final.append(P.dma("pool", out_d[i * 128:(i + 1) * 128, :], h2[:], reads=[H], stream="o"))

        for it in range(ntiles + 2):
            g3 = stage_S3(it - 2) if it - 2 >= 0 else None
            g2 = stage_S2(it - 1) if 0 <= it - 1 < ntiles else None
            if it < ntiles:
                stage_X(it, g3, g2)
                rem = it - it // 3
                n2 = max(1, (rem + NBIS - 3) // (NBIS - 2))
                stage_Y(it, g3, g2, n2)
                stage_W(it)
            else:
                drain(g3)
                drain(g2)

        if do_moe:
            last = []
            for e, L in P.L.items():
                for it in reversed(L):
                    if isinstance(it, tuple):
                        last.append(it[1])
                        break
            BAR = Buf()
            for t in last:
                BAR.w[id(t.sem)] = t
            for nm in ("h2_d", "h2T_d"):
                for k, t in bufof(nm).w.items():
                    BAR.w[k] = t
            TB = 1024
            ntok = ntiles * 128
            nblk = (ntok + TB - 1) // TB
            o = 0
            wgb, wub, wdb, stgb = [], [], [], []
            for par in range(2):
                wgb.append(arena[:, o:o + 2048].rearrange("p (k f) -> p k f", k=KC)); o += 2048
                wub.append(arena[:, o:o + 2048].rearrange("p (k f) -> p k f", k=KC)); o += 2048
                wdb.append(arena[:, o:o + 2048].rearrange("p (k f) -> p k f", k=2)); o += 2048
            for j in range(3):
                stgb.append(arena[:, o:o + 4096].bitcast(F32)); o += 4096
            accv = arena[:, o:o + 16384].bitcast(F32).rearrange("p (a d) -> p a d", a=8); o += 16384
            hblk = arena[:, o:o + 8192].rearrange("p (k t) -> p k t", k=KC); o += 8192
            sgt, hid = [], []
            for j in range(2):
                sgt.append(arena[:, o:o + 512]); o += 512
            for j in range(3):
                hid.append(arena[:, o:o + 1024].rearrange("p (f t) -> p f t", f=2)); o += 1024
            assert o <= A_END, (o, A_END)
            P.dma("sp", lng[:], ln2g_d, reads=[BAR], writes=[bufof("lng")], stream="lg")
            P.dma("sp", lnb[:], ln2b_d, reads=[BAR], writes=[bufof("lnb")], stream="lb")

            items = []
            for blk in range(nblk):
                t0 = blk * TB
                tb = min(TB, ntok - t0)
                for e in range(nexp):
                    for sub in range(0, tb, 512):
                        items.append((blk, e, t0, sub, min(512, tb - sub)))
            eseq = []
            for it in items:
                if not eseq or eseq[-1] != (it[0], it[1]):
                    eseq.append((it[0], it[1]))
            item_j = []
            jj = -1
            prev = None
            for it in items:
                if (it[0], it[1]) != prev:
                    jj += 1
                    prev = (it[0], it[1])
                item_j.append(jj)
            srcs = (wg_d, wu_d, wd_d)

            def emit_stage(j):
                if j >= len(eseq):
                    return
                e = eseq[j][1]
                for m in range(3):
                    kk = 8 if m < 2 else 2
                    P.dma("sp", stgb[m][:].rearrange("p (k f) -> p k f", k=kk), srcs[m][e], reads=[BAR],
                          writes=[bufof("stgb%d" % m)], stream="ws%d" % m)

            def emit_cast(j):
                if j >= len(eseq):
                    return
                par = j % 2
                dsts = (wgb[par], wub[par], wdb[par])
                for m in range(3):
                    WB = bufof("wb%d_%d" % (m, par))
                    if m < 2:
                        P.op("act", lambda eng, d=dsts[m], m=m: eng.copy(out=d.rearrange("p k f -> p (k f)"), in_=stgb[m][:]),
                             reads=[bufof("stgb%d" % m), BAR], writes=[WB])
                    else:
                        P.op("dve", lambda eng, d=dsts[m], m=m: eng.tensor_copy(out=d.rearrange("p k f -> p (k f)"), in_=stgb[m][:]),
                             reads=[bufof("stgb%d" % m), BAR], writes=[WB])

            gu_banks = ((0, 1), (2, 3))
            gu_n = [0]
            d_n = [0]
            rl_n = [0]
            loaded_blk = [-1]

            def emit_GU(s, f):
                if s >= len(items):
                    return
                blk, e, t0, sub, sw = items[s]
                par = item_j[s] % 2
                hh_ = sub // 512
                HBK = bufof("hblk%d" % hh_)
                if f == 0 and e == 0:
                    P.dma("sp", hblk[:, :, sub:sub + sw], h2T_d[:, :, t0 + sub:t0 + sub + sw], reads=[BAR], writes=[HBK], stream="hb%d" % hh_)
                pg, pu = gu_banks[gu_n[0] % 2]
                sp_ = gu_n[0] % 2
                gu_n[0] += 1
                hb_ = s % 3
                WG, WU = bufof("wb0_%d" % par), bufof("wb1_%d" % par)
                for kc in range(KC):
                    P.op("pe", lambda eng, kc=kc: eng.matmul(psum[pg][:, 0:sw], lhsT=wgb[par][:, kc, f * 128:(f + 1) * 128],
                                                             rhs=hblk[:, kc, sub:sub + sw], start=(kc == 0), stop=(kc == KC - 1)),
                         reads=[WG, HBK, BAR], writes=[pbuf[pg]], acc=(kc > 0))
                for kc in range(KC):
                    P.op("pe", lambda eng, kc=kc: eng.matmul(psum[pu][:, 0:sw], lhsT=wub[par][:, kc, f * 128:(f + 1) * 128],
                                                             rhs=hblk[:, kc, sub:sub + sw], start=(kc == 0), stop=(kc == KC - 1)),
                         reads=[WU, HBK, BAR], writes=[pbuf[pu]], acc=(kc > 0))
                SG = bufof("sgt%d" % sp_)
                P.op("act", lambda eng: eng.activation(out=sgt[sp_][:, 0:sw], in_=psum[pg][:, 0:sw], func=AF.Silu),
                     reads=[pbuf[pg], BAR], writes=[SG])
                P.op("dve", lambda eng: eng.tensor_tensor(out=hid[hb_][:, f, 0:sw], in0=sgt[sp_][:, 0:sw], in1=psum[pu][:, 0:sw], op=ALU.mult),
                     reads=[SG, pbuf[pu], BAR], writes=[bufof("hid%d_%d" % (hb_, f))])

            def emit_D(s):
                blk, e, t0, sub, sw = items[s]
                par = item_j[s] % 2
                hb_ = s % 3
                WD = bufof("wb2_%d" % par)
                g = 0
                for tt in range(sw // 128):
                    ta = (sub // 128) + tt
                    tg = t0 // 128 + ta
                    for n in range(2):
                        pd = 4 + d_n[0] % 4
                        d_n[0] += 1
                        for f in range(2):
                            P.op("pe", lambda eng, f=f, tt=tt, n=n, pd=pd: eng.matmul(
                                psum[pd][:], lhsT=hid[hb_][:, f, tt * 128:(tt + 1) * 128], rhs=wdb[par][:, f, n * 512:(n + 1) * 512],
                                start=(f == 0), stop=(f == 1)),
                                reads=[bufof("hid%d_%d" % (hb_, f)), WD], writes=[pbuf[pd]], acc=(f > 0))
                        ACC = bufof("acc%d_%d" % (ta, n))
                        av = accv[:, ta, n * 512:(n + 1) * 512]
                        gsc = gtall[:, tg, e:e + 1]
                        if g not in (1, 4, 7):
                            P.op("dve", lambda eng, pd=pd, av=av, gsc=gsc: eng.scalar_tensor_tensor(out=av, in0=psum[pd][:], scalar=gsc, in1=av,
                                                                                                   op0=ALU.mult, op1=ALU.add),
                                 reads=[pbuf[pd], bufof("gtall"), BAR], writes=[ACC])
                        else:
                            r = rl_n[0] % NRL
                            rl_n[0] += 1
                            RL = bufof("rl%d" % r)
                            P.op("act", lambda eng, pd=pd, r=r, gsc=gsc: eng.activation(out=rl[r][:], in_=psum[pd][:], func=AF.Copy, scale=gsc),
                                 reads=[pbuf[pd], bufof("gtall"), BAR], writes=[RL])
                            P.op("pool", lambda eng, r=r, av=av: eng.tensor_add(out=av, in0=av, in1=rl[r][:]),
                                 reads=[RL, BAR], writes=[ACC])
                        g += 1

            def emit_acc_init(blk, ta):
                t0 = blk * TB
                if t0 + ta * 128 >= ntok:
                    return
                tg = t0 // 128 + ta
                ACC = [bufof("acc%d_0" % ta), bufof("acc%d_1" % ta)]
                P.dma("sp", accv[:, ta, :], h2_d[tg * 128:(tg + 1) * 128, :], reads=[BAR], writes=ACC, stream="ai%d" % ta)

            def emit_block_final(blk, half):
                t0 = blk * TB
                tb = min(TB, ntok - t0)
                ta0 = half * 4
                ta1 = min(tb // 128, ta0 + 4)
                ntt = ta1 - ta0
                if ntt <= 0:
                    return
                MV8, RS8 = bufof("mv8_%d" % half), bufof("rs8_%d" % half)
                for ta in range(ta0, ta1):
                    ACC0, ACC1 = bufof("acc%d_0" % ta), bufof("acc%d_1" % ta)
                    tg = t0 // 128 + ta
                    if "ffn" in dbg_d:
                        final.append(P.dma("pool", dbg_d["ffn"][tg * 128:(tg + 1) * 128, :], accv[:, ta, :], reads=[ACC0, ACC1], stream="o"))
                    src = accv[:, ta, :]
                    for c in range(2):
                        P.op("dve", lambda eng, c=c, src=src: eng.bn_stats(out=stats[:, c, :], in_=src[:, c * 512:(c + 1) * 512]),
                             reads=[ACC0 if c == 0 else ACC1], writes=[bufof("stats")], acc=(c > 0))
                    P.op("dve", lambda eng, ta=ta: eng.bn_aggr(out=mv8[:, ta, :], in_=stats[:].rearrange("p a s -> p (a s)")),
                         reads=[bufof("stats")], writes=[MV8], acc=(ta > ta0))
                P.op("act", lambda eng: eng.activation(out=rs8[:, ta0:ta1], in_=mv8[:, ta0:ta1, 1], func=AF.Sqrt, bias=eps2t[:], scale=1.0),
                     reads=[MV8, BW], writes=[RS8])
                P.op("dve", lambda eng: eng.reciprocal(out=rs8[:, ta0:ta1], in_=rs8[:, ta0:ta1]), writes=[RS8])
                P.op("dve", lambda eng: eng.scalar_tensor_tensor(out=rs8[:, 8 + ta0:8 + ta1], in0=mv8[:, ta0:ta1, 0], scalar=-1.0, in1=rs8[:, ta0:ta1],
                                                                 op0=ALU.mult, op1=ALU.mult),
                     reads=[MV8], writes=[RS8])
                for ta in range(ta0, ta1):
                    ACC0, ACC1 = bufof("acc%d_0" % ta), bufof("acc%d_1" % ta)
                    tg = t0 // 128 + ta
                    xb = ta % 2
                    HH = bufof("htok%d" % xb)
                    src = accv[:, ta, :]
                    P.op("act", lambda eng, xb=xb, src=src, ta=ta: eng.activation(out=htok[xb][:], in_=src, func=AF.Identity,
                                                                                 bias=rs8[:, 8 + ta:9 + ta], scale=rs8[:, ta:ta + 1]),
                         reads=[ACC0, ACC1, RS8], writes=[HH])
                    if blk + 1 < nblk:
                        emit_acc_init(blk + 1, ta)
                    P.op("dve", lambda eng, xb=xb: eng.tensor_mul(out=htok[xb][:], in0=htok[xb][:], in1=lng[:]),
                         reads=[bufof("lng")], writes=[HH])
                    P.op("dve", lambda eng, xb=xb: eng.tensor_add(out=htok[xb][:], in0=htok[xb][:], in1=lnb[:]),
                         reads=[bufof("lnb")], writes=[HH])
                    final.append(P.dma("pool", out_d[tg * 128:(tg + 1) * 128, :], htok[xb][:], reads=[HH], stream="o%d" % xb))

            for ta in range(TB // 128):
                emit_acc_init(0, ta)
            emit_stage(0)
            emit_cast(0)
            emit_stage(1)
            emit_cast(1)
            emit_stage(2)
            emit_GU(0, 0)
            emit_GU(0, 1)
            emit_GU(1, 0)
            N = len(items)
            for s in range(N):
                emit_D(s)
                last_of_expert = (s == N - 1) or (item_j[s + 1] != item_j[s])
                if last_of_expert:
                    j = item_j[s]
                    emit_cast(j + 2)
                    emit_stage(j + 3)
                if items[s][1] == nexp - 1:
                    emit_block_final(items[s][0], items[s][3] // 512)
                emit_GU(s + 1, 1)
                emit_GU(s + 2, 0)

        fw = {}
        for t in final:
            k = id(t.sem)
            if k not in fw or fw[k].val < t.val:
                fw[k] = t
        P.finalize(list(fw.values()))
        print("ops traced:", P.nops, flush=True)
    return nc


def host_inputs(inputs):
    f = np.float32
    bf = ml_dtypes.bfloat16
    w_in = np.asarray(inputs["w_in"][0], dtype=f)
    o = np.cumsum([0, 512, 512, 64, 64, 256, 32, 8, 2048])
    c_up, c_q, c_k, c_v, c_iq, c_ik, c_iw, c_g = [slice(o[j], o[j + 1]) for j in range(8)]
    w_tok = np.ascontiguousarray(np.concatenate([w_in[:, c_up], w_in[:, c_g], w_in[:, c_v], w_in[:, c_iw]], axis=1))
    w_feat = np.ascontiguousarray(np.concatenate([w_in[:, c_q], w_in[:, c_k], w_in[:, c_iq]] + [w_in[:, c_ik]] * 3, axis=1))

    def rep(v):
        v = np.asarray(v, dtype=f).reshape(1, -1)
        return np.ascontiguousarray(np.broadcast_to(v, (128, v.shape[1])))

    def col(v):
        return np.ascontiguousarray(np.asarray(v, dtype=f).reshape(KC, 128).T)

    band = np.zeros((128, 12, 128), dtype=f)
    tp = np.arange(128)[:, None]
    tq = np.arange(128)[None, :]
    for g, w in enumerate((2, 4, 8, 16)):
        cur = ((tp <= tq) & (tp > tq - w)).astype(f) / w - (tp == tq).astype(f)
        prev = ((tp - 128) > (tq - w)).astype(f) / w
        cnt = np.minimum(w, tq + 1).astype(f)
        cur0 = ((tp <= tq) & (tp > tq - w)).astype(f) / cnt - (tp == tq).astype(f)
        band[:, g], band[:, 4 + g], band[:, 8 + g] = cur, prev, cur0
    cbias = np.where(tq <= tp, 0.0, -1e30).astype(f)
    pow2 = np.broadcast_to((2.0 ** -np.arange(NBIS + 1)).astype(f)[None, :], (128, NBIS + 1))
    common = {
        "w_in_tok": w_tok, "w_in_feat": w_feat,
        "ln_in_g": rep(inputs["ln_in_g"]), "ln_in_b": rep(inputs["ln_in_b"]),
        "ln1_g": rep(inputs["ln1_g"][0]), "ln1_b": rep(inputs["ln1_b"][0]),
        "ln2_g": rep(inputs["ln2_g"][0]), "ln2_b": rep(inputs["ln2_b"][0]),
        "ln_in_gc": col(inputs["ln_in_g"]), "ln_in_bc": col(inputs["ln_in_b"]),
        "ln1_gc": col(inputs["ln1_g"][0]), "ln1_bc": col(inputs["ln1_b"][0]),
        "ident": np.eye(128, dtype=f).astype(bf),
        "i4": np.ascontiguousarray(np.tile(np.eye(128, dtype=f), (1, 4))).astype(bf),
        "band": np.ascontiguousarray(band.reshape(128, 12 * 128)).astype(bf),
        "cbias": np.ascontiguousarray(cbias),
        "pow2": np.ascontiguousarray(pow2),
        "b_gate": np.ascontiguousarray(np.asarray(inputs["b_gate"], dtype=f).reshape(1, 2048)),
        "pool_w": np.ascontiguousarray(np.asarray(inputs["pool_w"][0], dtype=f)),
        "pool_scale": np.ascontiguousarray(np.asarray(inputs["pool_scale"][0], dtype=f).reshape(4, 128).T),
        "w_proj_pool": np.ascontiguousarray(np.asarray(inputs["w_proj_pool"][0], dtype=f)),
        "w_proj_attn": np.ascontiguousarray(np.asarray(inputs["w_proj_attn"][0], dtype=f)),
        "w_out": np.ascontiguousarray(np.asarray(inputs["w_out"][0], dtype=f)),
        "w_gr": np.ascontiguousarray(np.concatenate([np.asarray(inputs["w_group"][0], dtype=f),
                                                     np.asarray(inputs["w_router"][0], dtype=f)], axis=1)),
        "b_gr": rep(np.concatenate([np.asarray(inputs["b_group"][0], dtype=f), np.asarray(inputs["b_router"][0], dtype=f)])),
        "w_gate": np.ascontiguousarray(np.asarray(inputs["w_gate"][0], dtype=f)),
        "w_up": np.ascontiguousarray(np.asarray(inputs["w_up"][0], dtype=f)),
        "w_down": np.ascontiguousarray(np.asarray(inputs["w_down"][0], dtype=f)),
    }
    maps = []
    for c in range(8):
        m = dict(common)
        m["x"] = np.ascontiguousarray(np.asarray(inputs["x"][c], dtype=f))
        maps.append(m)
    return maps


def kernel(**inputs):
    nc = build({})
    maps = host_inputs(inputs)
    res = run_bass_kernel_spmd(nc, maps, core_ids=list(range(8)))
    return np.stack([r["out"] for r in res.results], axis=0).astype(np.float32)
```

```python
import numpy as np
import ml_dtypes
from contextlib import ExitStack
import concourse.bass as bass
import concourse.mybir as mybir
from concourse.bass_utils import run_bass_kernel_spmd

F32 = mybir.dt.float32
BF16 = mybir.dt.bfloat16
AF = mybir.ActivationFunctionType
ALU = mybir.AluOpType
AX = mybir.AxisListType

T = 4096
NT = 32
D = 1024
KC = 8
ALPHA = 2.0 ** 0.25
EPS = 1e-5
SM_SCALE = 0.125
NSEL = 256
NBIS = 20
CTOK = 512 + 2048 + 64 + 8
CFEAT = 512 + 64 + 256 + 96
NEG = -30000.0
ACT_SPLIT_MIN = 1536


class Tok:
    __slots__ = ("sem", "val", "need", "dma")


class Buf:
    def __init__(self):
        self.w = {}
        self.r = {}


class Prog:
    def __init__(self, nc, es):
        self.nc = nc
        self.es = es
        self.L = {e: [] for e in ("pe", "act", "dve", "pool", "sp")}
        self.esem = {e: es.enter_context(nc.semaphore("s_" + e)) for e in self.L}
        self.dsem = {}
        self.dcnt = {}
        self.nops = 0

    def _deps(self, reads, writes, extra):
        deps = {}
        for b in reads:
            for k, t in b.w.items():
                deps[(k, t.val if t.dma else id(t))] = t
        for b in writes:
            for k, t in b.w.items():
                deps[(k, t.val if t.dma else id(t))] = t
            for k, t in b.r.items():
                deps[(k, t.val if t.dma else id(t))] = t
        for t in extra:
            if t is not None:
                deps[(id(t.sem), id(t))] = t
        return list(deps.values())

    def _post(self, t, reads, writes, acc):
        k = id(t.sem)
        for b in reads:
            b.r[k] = t
        for b in writes:
            if not acc:
                b.w = {}
            b.r = {}
            b.w[k] = t

    def op(self, eng, fn, reads=(), writes=(), acc=False, extra=()):
        L = self.L[eng]
        for d in self._deps(reads, writes, extra):
            if eng == "pe" and d.sem is self.esem["pe"]:
                continue
            d.need = True
            L.append(d)
        t = Tok()
        t.sem = self.esem[eng]
        t.val = None
        t.need = False
        t.dma = False
        L.append((fn, t))
        self._post(t, reads, writes, acc)
        self.nops += 1
        return t

    def dma(self, eng, out, in_, reads=(), writes=(), acc=False, extra=(), stream="d"):
        if stream not in self.dsem:
            self.dsem[stream] = self.es.enter_context(self.nc.semaphore("d_" + stream))
            self.dcnt[stream] = 0
        L = self.L[eng]
        for d in self._deps(reads, writes, extra):
            d.need = True
            L.append(d)
        self.dcnt[stream] += 16
        t = Tok()
        t.sem = self.dsem[stream]
        t.val = self.dcnt[stream]
        t.need = True
        t.dma = True
        L.append((lambda e: e.dma_start(out=out, in_=in_), t))
        self._post(t, reads, writes, acc)
        return t

    def finalize(self, final_waits):
        for e, L in self.L.items():
            c = 0
            for it in L:
                if isinstance(it, tuple):
                    fn, t = it
                    if (not t.dma) and t.need:
                        c += 1
                        t.val = c
        nc = self.nc

        def replay(e, eng):
            waited = {}
            for it in self.L[e]:
                if isinstance(it, Tok):
                    k = id(it.sem)
                    if waited.get(k, 0) >= it.val:
                        continue
                    eng.wait_ge(it.sem, it.val)
                    waited[k] = it.val
                else:
                    fn, t = it
                    ins = fn(eng)
                    if t.dma:
                        ins.then_inc(t.sem, 16)
                    elif t.need:
                        ins.then_inc(t.sem, 1)
            if e == "sp":
                for t in final_waits:
                    eng.wait_ge(t.sem, t.val)

        with nc.Block() as block:
            @block.tensor
            def _(eng):
                replay("pe", eng)

            @block.scalar
            def _(eng):
                replay("act", eng)

            @block.vector
            def _(eng):
                replay("dve", eng)

            @block.gpsimd
            def _(eng):
                replay("pool", eng)

            @block.sync
            def _(eng):
                replay("sp", eng)


class PsumPool:
    def __init__(self, banks):
        self.banks = banks
        self.i = 0

    def get(self):
        b = self.banks[self.i % len(self.banks)]
        self.i += 1
        return b


def build(cfg):
    ntiles = cfg.get("ntiles", NT)
    dbg = cfg.get("dbg", ())
    do_moe = cfg.get("moe", True)
    nexp = cfg.get("nexp", 32)
    nc = bass.Bass("TRN2", target_bir_lowering=False)
    es = ExitStack()
    with es:
        P = Prog(nc, es)

        def dram_in(name, shape, dt=F32):
            return nc.dram_tensor(name, list(shape), dt, kind="ExternalInput").ap()

        def dram_out(name, shape, dt=F32):
            return nc.dram_tensor(name, list(shape), dt, kind="ExternalOutput").ap()

        def sb(name, shape, dt):
            return es.enter_context(nc.sbuf_tensor("sb_" + name, list(shape), dt))

        B_ = {}

        def bufof(name):
            if name not in B_:
                B_[name] = Buf()
            return B_[name]

        x_d = dram_in("x", [T, D])
        wtok_d = dram_in("w_in_tok", [D, CTOK]).rearrange("(kc p) c -> p kc c", p=128)
        wfeat_d = dram_in("w_in_feat", [D, CFEAT]).rearrange("(kc p) c -> p kc c", p=128)
        lng_d = dram_in("ln_in_g", [128, D])
        lnb_d = dram_in("ln_in_b", [128, D])
        ln1g_d = dram_in("ln1_g", [128, D])
        ln1b_d = dram_in("ln1_b", [128, D])
        ln2g_d = dram_in("ln2_g", [128, D])
        ln2b_d = dram_in("ln2_b", [128, D])
        lngc_d = dram_in("ln_in_gc", [128, KC])
        lnbc_d = dram_in("ln_in_bc", [128, KC])
        ln1gc_d = dram_in("ln1_gc", [128, KC])
        ln1bc_d = dram_in("ln1_bc", [128, KC])
        ident_d = dram_in("ident", [128, 128], BF16)
        i4_d = dram_in("i4", [128, 512], BF16)
        band_d = dram_in("band", [128, 12 * 128], BF16)
        cbias_d = dram_in("cbias", [128, 128])
        pow2_d = dram_in("pow2", [128, NBIS + 1])
        bgate_d = dram_in("b_gate", [1, 2048])
        poolw_d = dram_in("pool_w", [4, 128, 128]).rearrange("g d e -> d g e")
        pscale_d = dram_in("pool_scale", [128, 4])
        wpp_d = dram_in("w_proj_pool", [512, D]).rearrange("(g p) c -> p g c", p=128)
        wpa_d = dram_in("w_proj_attn", [512, D]).rearrange("(g p) c -> p g c", p=128)
        wout_d = dram_in("w_out", [D, D]).rearrange("(g p) c -> p g c", p=128)
        wr_d = dram_in("w_gr", [D, 36]).rearrange("(g p) c -> p g c", p=128)
        br_d = dram_in("b_gr", [128, 36])
        wg_d = dram_in("w_gate", [32, D, 256]).rearrange("e (kc p) f -> e p kc f", p=128)
        wu_d = dram_in("w_up", [32, D, 256]).rearrange("e (kc p) f -> e p kc f", p=128)
        wd_d = dram_in("w_down", [32, 256, D]).rearrange("e (fc p) d -> e p fc d", p=128)
        out_d = dram_out("out", [T, D])
        h2_d = nc.dram_tensor("h2_scr", [T, D], F32, kind="Internal").ap()
        h2T_d = nc.dram_tensor("h2T_scr", [128, KC, T], BF16, kind="Internal").ap()
        hscr_d = nc.dram_tensor("h_scr", [T, D], F32, kind="Internal").ap()
        dbg_d = {}
        for name, shape in dbg:
            dbg_d[name] = dram_out("dbg_" + name, shape)

        A_WTOK, A_WFEAT, A_WOUT = 0, KC * CTOK, KC * CTOK + KC * CFEAT
        A_WPP = A_WOUT + KC * D
        A_WPA = A_WPP + 4 * D
        A_SC = A_WPA + 4 * D
        A_MB = A_SC + 2 * T
        A_END = A_MB + T
        arena = sb("arena", [128, A_END], BF16)
        wtok = arena[:, A_WTOK:A_WFEAT].rearrange("p (k c) -> p k c", k=KC)
        wfeat = arena[:, A_WFEAT:A_WOUT].rearrange("p (k c) -> p k c", k=KC)
        wout = arena[:, A_WOUT:A_WPP].rearrange("p (k c) -> p k c", k=KC)
        wpp = arena[:, A_WPP:A_WPA].rearrange("p (k c) -> p k c", k=4)
        wpa = arena[:, A_WPA:A_SC].rearrange("p (k c) -> p k c", k=4)
        scores = arena[:, A_SC:A_MB].bitcast(F32)
        mb = arena[:, A_MB:A_END]

        ident = sb("ident", [128, 128], BF16)
        i4 = sb("i4", [128, 512], BF16)
        band = sb("band", [128, 12, 128], BF16)
        cbias = sb("cbias", [128, 128], F32)
        pow2 = sb("pow2", [128, NBIS + 1], F32)
        lncol = sb("lncol", [128, 4, KC], F32)
        epst = sb("epst", [128, 1], F32)
        eps2t = sb("eps2t", [128, 1], F32)
        ones1 = sb("ones1", [1, 128], BF16)
        bgate = sb("bgate", [1, 2048], BF16)
        poolw = sb("poolw", [128, 4, 128], BF16)
        pscale = sb("pscale", [128, 4], F32)
        wr = sb("wr", [128, KC, 36], BF16)
        brt = sb("brt", [128, 36], F32)
        lng = sb("lng", [128, D], F32)
        lnb = sb("lnb", [128, D], F32)
        ln1g = sb("ln1g", [128, D], F32)
        ln1b = sb("ln1b", [128, D], F32)
        htok = [sb("htok%d" % i, [128, D], F32) for i in range(2)]
        hbf = sb("hbf", [128, D], BF16)
        hT = sb("hT", [128, KC, 128], BF16)
        stats = sb("stats", [128, 2, 6], F32)
        mv = sb("mv", [128, 2], F32)
        rstd = sb("rstd", [128, 1], F32)
        mv8 = sb("mv8", [128, 8, 2], F32)
        rs8 = sb("rs8", [128, 16], F32)
        nmr = sb("nmr", [128, 1], F32)
        upool = [sb("upool%d" % i, [128, 512], BF16) for i in range(2)]
        gates = [sb("gates%d" % i, [128, 2048], BF16) for i in range(2)]
        kT = sb("kT", [64, T], BF16)
        ikT4 = sb("ikT4", [128, T], BF16)
        vaug = sb("vaug", [128, NT, 65], BF16)
        qT = [sb("qT%d" % i, [64, 8 * 128], BF16) for i in range(2)]
        iqT = sb("iqT", [128, 3, 128], BF16)
        iwt = sb("iwt", [128, 8], F32)
        NRL = 4
        rl = [sb("rl%d" % i, [128, 512], F32) for i in range(NRL)]
        junk1 = sb("junk1", [128, 1], BF16)
        junk = junk1[:, 0:1].to_broadcast([128, T])
        junk2t = sb("junk2", [128, 1], BF16)
        junk2 = junk2t[:, 0:1].to_broadcast([128, T])
        sga = sb("sga", [128, 1], F32)
        mabs = sb("mabs", [128, 1], F32)
        wk = sb("wk", [128, NBIS + 1], F32)
        tcur = sb("tcur", [128, 1], F32)
        cnt = sb("cnt", [128, 1], F32)
        sgn = sb("sgn", [128, 1], F32)
        thr = sb("thr", [128, 1], F32)
        PT0 = sb("PT0", [128, 1024], BF16)
        PT = [PT0, PT0]
        rden = sb("rden", [128, 8], F32)
        hbf2 = sb("hbf2", [128, D], BF16)
        hT2 = sb("hT2", [128, KC, 128], BF16)
        ao = hbf2[:, 0:512]
        aoT = hT2[:, 0:4, :]
        deltaT = sb("deltaT", [128, 4, 128], BF16)
        poT = [sb("poT%d" % i, [128, 4, 128], BF16) for i in range(2)]
        mg = hbf2
        mgT = hT2
        h2bf = hbf2
        h2T = hT2
        rt = sb("rt", [128, 36], F32)
        rsm = sb("rsm", [128, 16], F32)
        m8 = sb("m8", [128, 8], F32)
        g1t = sb("g1t", [128, 32], F32)
        g2t = sb("g2t", [128, 32], F32)
        gtall = sb("gtall", [128, NT, 32], F32)

        psum = [es.enter_context(nc.psum_tensor("ps%d" % i, [128, 512], F32)) for i in range(8)]
        pbuf = [Buf() for _ in range(8)]
        gen = PsumPool(list(range(0, 6)))

        BW = bufof("w")
        P.dma("sp", ident[:], ident_d, writes=[BW], acc=True, stream="c")
        P.dma("sp", i4[:], i4_d, writes=[BW], acc=True, stream="c")
        P.dma("sp", band[:].rearrange("p a t -> p (a t)"), band_d, writes=[BW], acc=True, stream="c")
        P.dma("sp", cbias[:], cbias_d, writes=[BW], acc=True, stream="c")
        P.dma("sp", pow2[:], pow2_d, writes=[BW], acc=True, stream="c")
        P.dma("sp", pscale[:], pscale_d, writes=[BW], acc=True, stream="c")
        P.dma("sp", brt[:], br_d, writes=[BW], acc=True, stream="c")
        for j, dd in enumerate((lngc_d, lnbc_d, ln1gc_d, ln1bc_d)):
            P.dma("sp", lncol[:, j, :], dd, writes=[BW], acc=True, stream="c")
        P.dma("sp", lng[:], lng_d, writes=[bufof("lng")], stream="lg")
        P.dma("sp", lnb[:], lnb_d, writes=[bufof("lnb")], stream="lb")
        P.dma("sp", ln1g[:], ln1g_d, writes=[BW], acc=True, stream="c")
        P.dma("sp", ln1b[:], ln1b_d, writes=[BW], acc=True, stream="c")
        P.op("dve", lambda eng: eng.memset(epst[:], EPS), writes=[BW], acc=True)
        P.op("dve", lambda eng: eng.memset(eps2t[:], EPS / (ALPHA * ALPHA)), writes=[BW], acc=True)
        P.op("dve", lambda eng: eng.memset(ones1[:], 1.0), writes=[BW], acc=True)
        P.op("pool", lambda eng: eng.memset(vaug[:].rearrange("p a c -> p (a c)"), 1.0), writes=[bufof("vaug")])

        cast_n = [0]

        last_cast = {}

        def stage_cast(src, dst, rows, cols):
            for c0 in range(0, cols, 1024):
                c1 = min(cols, c0 + 1024)
                n = cast_n[0]
                cast_n[0] += 1
                j = n % 4
                s_ = scores[:, j * 1024:(j + 1) * 1024]
                sbf = bufof("stgS%d" % j)
                P.dma("sp", s_[0:rows, 0:c1 - c0], src[:, c0:c1], writes=[sbf], stream="w%d" % j)
                e = ("act", "dve", "pool", "act")[n % 4]
                if e == "act":
                    t = P.op("act", lambda eng, d=dst, s_=s_, c0=c0, c1=c1: eng.copy(out=d[:, c0:c1], in_=s_[0:rows, 0:c1 - c0]),
                             reads=[sbf], writes=[BW], acc=True)
                else:
                    t = P.op(e, lambda eng, d=dst, s_=s_, c0=c0, c1=c1: eng.tensor_copy(out=d[:, c0:c1], in_=s_[0:rows, 0:c1 - c0]),
                             reads=[sbf], writes=[BW], acc=True)
                last_cast[e] = t

        for kc in range(KC):
            stage_cast(wtok_d[:, kc, :], wtok[:, kc, :], 128, CTOK)
            stage_cast(wfeat_d[:, kc, :], wfeat[:, kc, :], 128, CFEAT)
        for g in range(4):
            stage_cast(wpp_d[:, g, :], wpp[:, g, :], 128, D)
            stage_cast(wpa_d[:, g, :], wpa[:, g, :], 128, D)
            stage_cast(poolw_d[:, g, :], poolw[:, g, :], 128, 128)
        for kc in range(KC):
            stage_cast(wout_d[:, kc, :], wout[:, kc, :], 128, D)
            stage_cast(wr_d[:, kc, :], wr[:, kc, :], 128, 36)
        stage_cast(bgate_d, bgate[:], 1, 2048)
        for t in last_cast.values():
            bufof("scores").r[id(t.sem)] = t

        def layer_norm(src, dst, gt, bt, srcbuf, dstbuf, gbufs, bf=None, bfbuf=None):
            for c in range(2):
                P.op("dve", lambda eng, c=c: eng.bn_stats(out=stats[:, c, :], in_=src[:, c * 512:(c + 1) * 512]),
                     reads=[srcbuf], writes=[bufof("stats")], acc=(c > 0))
            P.op("dve", lambda eng: eng.bn_aggr(out=mv[:], in_=stats[:].rearrange("p a s -> p (a s)")),
                 reads=[bufof("stats")], writes=[bufof("mv")])
            P.op("act", lambda eng: eng.activation(out=rstd[:], in_=mv[:, 1:2], func=AF.Sqrt, bias=epst[:], scale=1.0),
                 reads=[bufof("mv"), BW], writes=[bufof("rstd")])
            P.op("dve", lambda eng: eng.reciprocal(out=rstd[:], in_=rstd[:]), writes=[bufof("rstd")])
            P.op("dve", lambda eng: eng.scalar_tensor_tensor(out=nmr[:], in0=mv[:, 0:1], scalar=-1.0, in1=rstd[:],
                                                             op0=ALU.mult, op1=ALU.mult),
                 reads=[bufof("mv"), bufof("rstd")], writes=[bufof("nmr")])
            if bf is not None:
                P.op("act", lambda eng: eng.activation(out=bf[:], in_=src[:], func=AF.Identity, bias=nmr[:], scale=rstd[:]),
                     reads=[srcbuf, bufof("rstd"), bufof("nmr")], writes=[bfbuf])
            P.op("act", lambda eng: eng.activation(out=dst[:], in_=src[:], func=AF.Identity, bias=nmr[:], scale=rstd[:]),
                 reads=[srcbuf, bufof("rstd"), bufof("nmr")], writes=[dstbuf])
            P.op("pool", lambda eng: eng.tensor_mul(out=dst[:], in0=dst[:], in1=gt[:]), reads=gbufs, writes=[dstbuf])
            P.op("pool", lambda eng: eng.tensor_add(out=dst[:], in0=dst[:], in1=bt[:]), reads=gbufs, writes=[dstbuf])

        def transpose_to(src_bf, nblk, dst_flat, srcbuf, dstbuf, gb=None):
            pb = gen.get()
            pT = psum[pb][:].bitcast(BF16)
            for kc in range(nblk):
                P.op("pe", lambda eng, kc=kc, pT=pT: eng.transpose(out=pT[:, kc * 128:(kc + 1) * 128],
                                                                   in_=src_bf[:, kc * 128:(kc + 1) * 128], identity=ident[:]),
                     reads=[srcbuf, BW], writes=[pbuf[pb]], acc=(kc > 0))
            if gb is None:
                P.op("dve", lambda eng, pT=pT: eng.tensor_copy(out=dst_flat, in_=pT[:, 0:nblk * 128]),
                     reads=[pbuf[pb]], writes=[dstbuf])
            else:
                for kc in range(nblk):
                    P.op("dve", lambda eng, pT=pT, kc=kc: eng.tensor_scalar(
                        out=dst_flat[:, kc * 128:(kc + 1) * 128], in0=pT[:, kc * 128:(kc + 1) * 128],
                        scalar1=lncol[:, gb, kc:kc + 1], scalar2=lncol[:, gb + 1, kc:kc + 1], op0=ALU.mult, op1=ALU.add),
                        reads=[pbuf[pb], BW], writes=[dstbuf], acc=(kc > 0))

        final = []
        HB, HT = bufof("hbf"), bufof("hT")
        SC, MB = bufof("scores"), bufof("mb")

        def dbg_store(name, i, src, srcbuf, width):
            if name in dbg_d:
                final.append(P.dma("pool", dbg_d[name][i * 128:(i + 1) * 128, 0:width], src, reads=[srcbuf], stream="o"))

        def advance(g, n=1):
            if g is None:
                return
            for _ in range(n):
                try:
                    next(g)
                except StopIteration:
                    return

        def drain(g):
            if g is None:
                return
            for _ in g:
                pass

        def stage_X(i, g3=None, g2=None):
            H = bufof("htok0")
            P.dma("sp", htok[0][:], x_d[i * 128:(i + 1) * 128, :], writes=[H], stream="x0")
            layer_norm(htok[0], htok[0], lng, lnb, H, H, [bufof("lng"), bufof("lnb")], bf=hbf, bfbuf=HB)
            P.dma("pool", hscr_d[i * 128:(i + 1) * 128, :], htok[0][:], reads=[H], writes=[bufof("hscr%d" % (i % 4))], stream="hs%d" % (i % 4))
            transpose_to(hbf, KC, hT[:].rearrange("p k t -> p (k t)"), HB, HT, gb=0)
            pb = gen.get()
            for kc in range(KC):
                P.op("pe", lambda eng, kc=kc, pb=pb: eng.matmul(psum[pb][:, 0:72], lhsT=hT[:, kc, :], rhs=wtok[:, kc, 2560:2632],
                                                                start=(kc == 0), stop=(kc == KC - 1)),
                     reads=[HT, BW], writes=[pbuf[pb]], acc=(kc > 0))
            P.op("dve", lambda eng, pb=pb: eng.tensor_copy(out=vaug[:, i, 0:64], in_=psum[pb][:, 0:64]),
                 reads=[pbuf[pb]], writes=[bufof("vaug")], acc=True)
            P.op("dve", lambda eng, pb=pb: eng.tensor_copy(out=iwt[:], in_=psum[pb][:, 64:72]),
                 reads=[pbuf[pb]], writes=[bufof("iwt")])
            pb = gen.get()
            fm = ((512, 64), (576, 96), (672, 96), (768, 64))
            for j, (c0, m) in enumerate(fm):
                for kc in range(KC):
                    P.op("pe", lambda eng, kc=kc, pb=pb, j=j, c0=c0, m=m: eng.matmul(
                        psum[pb][0:m, j * 128:(j + 1) * 128], lhsT=wfeat[:, kc, c0:c0 + m], rhs=hT[:, kc, :],
                        start=(kc == 0), stop=(kc == KC - 1)),
                        reads=[HT, BW], writes=[pbuf[pb]], acc=(kc > 0 or j > 0))
            P.op("dve", lambda eng, pb=pb: eng.tensor_copy(out=kT[:, i * 128:(i + 1) * 128], in_=psum[pb][0:64, 0:128]),
                 reads=[pbuf[pb]], writes=[bufof("kT")], acc=True)
            P.op("dve", lambda eng, pb=pb: eng.tensor_copy(out=iqT[0:96, 0:2, :].rearrange("p a t -> p (a t)"), in_=psum[pb][0:96, 128:384]),
                 reads=[pbuf[pb]], writes=[bufof("iqT")])
            P.op("dve", lambda eng, pb=pb: eng.tensor_copy(out=iqT[0:64, 2, :], in_=psum[pb][0:64, 384:512]),
                 reads=[pbuf[pb]], writes=[bufof("iqT")], acc=True)
            pb = gen.get()
            for kc in range(KC):
                P.op("pe", lambda eng, kc=kc, pb=pb: eng.matmul(psum[pb][0:96, 0:128], lhsT=wfeat[:, kc, 832:928], rhs=hT[:, kc, :],
                                                                start=(kc == 0), stop=(kc == KC - 1)),
                     reads=[HT, BW], writes=[pbuf[pb]], acc=(kc > 0))
            P.op("act", lambda eng, pb=pb: eng.copy(out=ikT4[0:96, i * 128:(i + 1) * 128], in_=psum[pb][0:96, 0:128]),
                 reads=[pbuf[pb]], writes=[bufof("ikT4")], acc=True)
            S = 128 * (i + 1)
            nch = (S + 511) // 512
            nrl = 0
            n_it = 8 * nch
            cx = i // 3
            every2 = max(1, n_it // (cx + 1))
            advance(g3, 1)
            for c in range(nch):
                w = min(512, S - c * 512)
                for h in range(8):
                    if nrl % 3 == 2:
                        advance(g3, 1)
                    if cx > 0 and nrl % every2 == every2 - 1:
                        advance(g2, 1)
                    pb = gen.get()
                    hp, hc = h % 3, h // 3
                    P.op("pe", lambda eng, pb=pb, hp=hp, hc=hc, c=c, w=w: eng.matmul(
                        psum[pb][:, 0:w], lhsT=iqT[hp * 32:(hp + 1) * 32, hc, :], rhs=ikT4[hp * 32:(hp + 1) * 32, c * 512:c * 512 + w],
                        start=True, stop=True),
                        reads=[bufof("iqT"), bufof("ikT4")], writes=[pbuf[pb]])
                    r = nrl % NRL
                    nrl += 1
                    RL = bufof("rl%d" % r)
                    P.op("act", lambda eng, pb=pb, r=r, w=w: eng.activation(out=rl[r][:, 0:w], in_=psum[pb][:, 0:w], func=AF.Relu),
                         reads=[pbuf[pb]], writes=[RL])
                    if h == 0:
                        P.op("dve", lambda eng, r=r, c=c, w=w: eng.tensor_scalar(
                            out=scores[:, c * 512:c * 512 + w], in0=rl[r][:, 0:w], scalar1=iwt[:, 0:1], scalar2=None, op0=ALU.mult),
                            reads=[RL, bufof("iwt")], writes=[SC], acc=(c > 0))
                    else:
                        P.op("dve", lambda eng, r=r, c=c, w=w, h=h: eng.scalar_tensor_tensor(
                            out=scores[:, c * 512:c * 512 + w], in0=rl[r][:, 0:w], scalar=iwt[:, h:h + 1],
                            in1=scores[:, c * 512:c * 512 + w], op0=ALU.mult, op1=ALU.add),
                            reads=[RL, bufof("iwt")], writes=[SC], acc=True)

        def stage_Y(i, g3=None, g2=None, n2=2):
            S = 128 * (i + 1)
            P.op("dve", lambda eng: eng.tensor_reduce(out=mabs[:], in_=scores[:, 0:S], axis=AX.X, op=ALU.max,
                                                      apply_absolute_value=True),
                 reads=[SC], writes=[bufof("mabs")])
            P.op("dve", lambda eng: eng.tensor_tensor(out=scores[:, i * 128:(i + 1) * 128], in0=scores[:, i * 128:(i + 1) * 128],
                                                      in1=cbias[:], op=ALU.add),
                 reads=[BW], writes=[SC])
            P.op("dve", lambda eng: eng.tensor_scalar(out=mabs[:], in0=mabs[:], scalar1=1.001, scalar2=1e-20, op0=ALU.mult, op1=ALU.add),
                 writes=[bufof("mabs")])
            P.op("dve", lambda eng: eng.tensor_scalar(out=wk[:], in0=pow2[:], scalar1=mabs[:, 0:1], scalar2=None, op0=ALU.mult),
                 reads=[bufof("mabs"), BW], writes=[bufof("wk")])
            P.op("dve", lambda eng: eng.memset(tcur[:], 0.0), writes=[bufof("tcur")])
            JK = bufof("junk")
            split = (S >= ACT_SPLIT_MIN)
            Sd = (S // 2) if split else S
            na = S - Sd
            for k in range(NBIS if S > NSEL else 0):
                P.op("dve", lambda eng: eng.tensor_scalar(out=junk[:, 0:Sd], in0=scores[:, 0:Sd], scalar1=tcur[:, 0:1], scalar2=0.0,
                                                          op0=ALU.is_ge, op1=ALU.add, accum_out=cnt[:]),
                     reads=[SC, bufof("tcur")], writes=[JK, bufof("cnt")])
                if split:
                    P.op("act", lambda eng: eng.activation(out=junk2[:, 0:na], in_=scores[:, Sd:S], func=AF.Sign, bias=tcur[:, 0:1],
                                                           scale=-1.0, accum_out=sga[:]),
                         reads=[SC, bufof("tcur")], writes=[bufof("junk2"), bufof("sga")])
                    P.op("dve", lambda eng: eng.scalar_tensor_tensor(out=cnt[:], in0=cnt[:], scalar=2.0, in1=sga[:],
                                                                     op0=ALU.mult, op1=ALU.subtract),
                         reads=[bufof("sga")], writes=[bufof("cnt")])
                    P.op("dve", lambda eng: eng.tensor_scalar(out=sgn[:], in0=cnt[:], scalar1=float(2 * NSEL - na), scalar2=-0.5,
                                                              op0=ALU.is_ge, op1=ALU.add),
                         reads=[bufof("cnt")], writes=[bufof("sgn")])
                else:
                    P.op("dve", lambda eng: eng.tensor_scalar(out=sgn[:], in0=cnt[:], scalar1=float(NSEL), scalar2=-0.5,
                                                              op0=ALU.is_ge, op1=ALU.add),
                         reads=[bufof("cnt")], writes=[bufof("sgn")])
                P.op("dve", lambda eng, k=k: eng.scalar_tensor_tensor(out=tcur[:], in0=sgn[:], scalar=wk[:, k:k + 1], in1=tcur[:],
                                                                      op0=ALU.mult, op1=ALU.add),
                     reads=[bufof("sgn"), bufof("wk")], writes=[bufof("tcur")])
                advance(g3, 1)
                advance(g2, n2)
            drain(g2)
            if S > NSEL:
                P.op("dve", lambda eng: eng.scalar_tensor_tensor(out=thr[:], in0=wk[:, NBIS:NBIS + 1], scalar=-1.0, in1=tcur[:],
                                                                 op0=ALU.mult, op1=ALU.add),
                     reads=[bufof("wk"), bufof("tcur")], writes=[bufof("thr")])
            else:
                P.op("dve", lambda eng: eng.tensor_scalar(out=thr[:], in0=wk[:, 0:1], scalar1=-1.0, scalar2=None, op0=ALU.mult),
                     reads=[bufof("wk")], writes=[bufof("thr")])
            P.op("dve", lambda eng: eng.tensor_scalar(out=mb[:, 0:S], in0=scores[:, 0:S], scalar1=thr[:, 0:1], scalar2=NEG,
                                                      op0=ALU.is_lt, op1=ALU.mult),
                 reads=[SC, bufof("thr")], writes=[MB])
            dbg_store("thr", i, thr[:], bufof("thr"), 1)
            drain(g3)
            drain(g2)

        def stage_W(i):
            ub = i % 2
            UP, UPP = bufof("upool%d" % ub), bufof("upool%d" % (1 - ub))
            GT, QT, POT = bufof("gates%d" % ub), bufof("qT%d" % ub), bufof("poT%d" % ub)
            pb = gen.get()
            for kc in range(KC):
                P.op("pe", lambda eng, kc=kc, pb=pb: eng.matmul(psum[pb][:], lhsT=hT[:, kc, :], rhs=wtok[:, kc, 0:512],
                                                                start=(kc == 0), stop=(kc == KC - 1)),
                     reads=[HT, BW], writes=[pbuf[pb]], acc=(kc > 0))
            P.op("act", lambda eng, pb=pb: eng.copy(out=upool[ub][:], in_=psum[pb][:]), reads=[pbuf[pb]], writes=[UP])
            for j in range(4):
                pb = gen.get()
                for kc in range(KC):
                    P.op("pe", lambda eng, kc=kc, pb=pb, j=j: eng.matmul(psum[pb][:], lhsT=hT[:, kc, :],
                                                                         rhs=wtok[:, kc, 512 + j * 512:1024 + j * 512],
                                                                         start=(kc == 0), stop=False),
                         reads=[HT, BW], writes=[pbuf[pb]], acc=(kc > 0))
                P.op("pe", lambda eng, pb=pb, j=j: eng.matmul(psum[pb][:], lhsT=ones1[0:1, :], rhs=bgate[0:1, j * 512:(j + 1) * 512],
                                                              start=False, stop=True),
                     reads=[BW], writes=[pbuf[pb]], acc=True)
                P.op("act", lambda eng, pb=pb, j=j: eng.activation(out=gates[ub][:, j * 512:(j + 1) * 512], in_=psum[pb][:], func=AF.Sigmoid),
                     reads=[pbuf[pb]], writes=[GT], acc=(j > 0))
            for half in range(2):
                pb = gen.get()
                for hh in range(4):
                    h = half * 4 + hh
                    for kc in range(KC):
                        P.op("pe", lambda eng, kc=kc, pb=pb, hh=hh, h=h: eng.matmul(
                            psum[pb][0:64, hh * 128:(hh + 1) * 128], lhsT=wfeat[:, kc, h * 64:(h + 1) * 64], rhs=hT[:, kc, :],
                            start=(kc == 0), stop=(kc == KC - 1)),
                            reads=[HT, BW], writes=[pbuf[pb]], acc=(kc > 0 or hh > 0))
                P.op("act", lambda eng, pb=pb, half=half: eng.copy(out=qT[ub][:, half * 512:(half + 1) * 512], in_=psum[pb][0:64, :]),
                     reads=[pbuf[pb]], writes=[QT], acc=(half > 0))
            pb = gen.get()
            for g in range(4):
                P.op("pe", lambda eng, pb=pb, g=g: eng.matmul(
                    psum[pb][:, g * 128:(g + 1) * 128], lhsT=upool[ub][:, g * 128:(g + 1) * 128],
                    rhs=band[:, (8 + g) if i == 0 else g, :], start=True, stop=(i == 0)),
                    reads=[UP, BW], writes=[pbuf[pb]], acc=(g > 0))
                if i > 0:
                    P.op("pe", lambda eng, pb=pb, g=g: eng.matmul(
                        psum[pb][:, g * 128:(g + 1) * 128], lhsT=upool[1 - ub][:, g * 128:(g + 1) * 128],
                        rhs=band[:, 4 + g, :], start=False, stop=True),
                        reads=[UPP, BW], writes=[pbuf[pb]], acc=True)
            P.op("act", lambda eng, pb=pb: eng.copy(out=deltaT[:].rearrange("p a t -> p (a t)"), in_=psum[pb][:]),
                 reads=[pbuf[pb]], writes=[bufof("deltaT")])
            pb = gen.get()
            for g in range(4):
                P.op("pe", lambda eng, pb=pb, g=g: eng.matmul(psum[pb][:, g * 128:(g + 1) * 128], lhsT=poolw[:, g, :],
                                                              rhs=deltaT[:, g, :], start=True, stop=True),
                     reads=[bufof("deltaT"), BW], writes=[pbuf[pb]], acc=(g > 0))
            for g in range(4):
                P.op("dve", lambda eng, pb=pb, g=g: eng.tensor_scalar(out=poT[ub][:, g, :], in0=psum[pb][:, g * 128:(g + 1) * 128],
                                                                      scalar1=pscale[:, g:g + 1], scalar2=None, op0=ALU.mult),
                     reads=[pbuf[pb], BW], writes=[POT], acc=(g > 0))

        def stage_S2(i):
            ub = i % 2
            QT = bufof("qT%d" % ub)
            for c in range(i + 1):
                pr = c % 2
                PTB = bufof("PT0")
                for half in range(2):
                    pb = gen.get()
                    P.op("pe", lambda eng, pb=pb, c=c, half=half: eng.matmul(
                        psum[pb][:], lhsT=kT[:, c * 128:(c + 1) * 128], rhs=qT[ub][:, half * 512:(half + 1) * 512], start=True, stop=False),
                        reads=[bufof("kT"), QT], writes=[pbuf[pb]])
                    P.op("pe", lambda eng, pb=pb, c=c: eng.matmul(
                        psum[pb][:], lhsT=mb[:, c * 128:(c + 1) * 128], rhs=i4[:], start=False, stop=True),
                        reads=[MB, BW], writes=[pbuf[pb]], acc=True)
                    P.op("act", lambda eng, pb=pb, pr=pr, half=half: eng.activation(
                        out=PT[pr][:, half * 512:(half + 1) * 512], in_=psum[pb][:], func=AF.Exp, scale=SM_SCALE),
                        reads=[pbuf[pb]], writes=[PTB], acc=(half > 0))
                for h in range(8):
                    ob = 6 + h // 4
                    P.op("pe", lambda eng, ob=ob, h=h, pr=pr, c=c: eng.matmul(
                        psum[ob][:, (h % 4) * 65:(h % 4) * 65 + 65], lhsT=PT[pr][:, h * 128:(h + 1) * 128], rhs=vaug[:, c, :],
                        start=(c == 0 and h % 4 == 0), stop=(c == i and h % 4 == 3)),
                        reads=[PTB, bufof("vaug")], writes=[pbuf[ob]], acc=not (c == 0 and h % 4 == 0))
                yield

        def stage_S3(i):
            ub = i % 2
            xb = 1
            H = bufof("htok1")
            GT, POT = bufof("gates%d" % ub), bufof("poT%d" % ub)
            HB, HT = bufof("hbf2"), bufof("hT2")
            P.dma("sp", htok[1][:], hscr_d[i * 128:(i + 1) * 128, :], reads=[bufof("hscr%d" % (i % 4))], writes=[H], stream="hl")
            for hb in range(2):
                ob = 6 + hb
                ov = psum[ob][:, 0:260].rearrange("p (h e) -> p h e", e=65)
                P.op("dve", lambda eng, ov=ov, hb=hb: eng.reciprocal(out=rden[:, hb * 4:(hb + 1) * 4], in_=ov[:, :, 64]),
                     reads=[pbuf[ob]], writes=[bufof("rden")], acc=(hb > 0))
                P.op("dve", lambda eng, ov=ov, hb=hb: eng.tensor_tensor(
                    out=ao[:, hb * 256:(hb + 1) * 256].rearrange("p (h e) -> p h e", e=64), in0=ov[:, :, 0:64],
                    in1=rden[:, hb * 4:(hb + 1) * 4].unsqueeze(2).to_broadcast([128, 4, 64]), op=ALU.mult),
                    reads=[pbuf[ob], bufof("rden")], writes=[HB], acc=(hb > 0))
            yield
            if "ao" in dbg_d:
                P.op("dve", lambda eng: eng.tensor_copy(out=rl[0][:], in_=ao), reads=[HB], writes=[bufof("rl0")])
                dbg_store("ao", i, rl[0][:], bufof("rl0"), 512)
            if "po" in dbg_d:
                P.op("dve", lambda eng: eng.tensor_copy(out=rl[1][:], in_=poT[ub][:].rearrange("p a t -> p (a t)")),
                     reads=[POT], writes=[bufof("rl1")])
                dbg_store("po", i, rl[1][:], bufof("rl1"), 512)
            transpose_to(ao, 4, aoT.rearrange("p a t -> p (a t)"), HB, HT)
            yield
            for n in range(2):
                pbp = gen.get()
                for g in range(4):
                    P.op("pe", lambda eng, pbp=pbp, g=g, n=n: eng.matmul(psum[pbp][:], lhsT=poT[ub][:, g, :], rhs=wpp[:, g, n * 512:(n + 1) * 512],
                                                                         start=(g == 0), stop=(g == 3)),
                         reads=[POT, BW], writes=[pbuf[pbp]], acc=(g > 0))
                pba = gen.get()
                for g in range(4):
                    P.op("pe", lambda eng, pba=pba, g=g, n=n: eng.matmul(psum[pba][:], lhsT=aoT[:, g, :], rhs=wpa[:, g, n * 512:(n + 1) * 512],
                                                                         start=(g == 0), stop=(g == 3)),
                         reads=[HT, BW], writes=[pbuf[pba]], acc=(g > 0))
                P.op("dve", lambda eng, pbp=pbp, n=n: eng.tensor_tensor(out=rl[0][:], in0=gates[ub][:, n * 512:(n + 1) * 512], in1=psum[pbp][:], op=ALU.mult),
                     reads=[GT, pbuf[pbp]], writes=[bufof("rl0")])
                P.op("dve", lambda eng, pba=pba, n=n: eng.tensor_tensor(out=rl[1][:], in0=gates[ub][:, 1024 + n * 512:1536 + n * 512], in1=psum[pba][:], op=ALU.mult),
                     reads=[GT, pbuf[pba]], writes=[bufof("rl1")])
                P.op("pool", lambda eng, n=n: eng.tensor_add(out=mg[:, n * 512:(n + 1) * 512], in0=rl[0][:], in1=rl[1][:]),
                     reads=[bufof("rl0"), bufof("rl1")], writes=[HB], acc=(n > 0))
                yield
            transpose_to(mg, KC, mgT[:].rearrange("p k t -> p (k t)"), HB, HT)
            yield
            for n in range(2):
                pb = gen.get()
                for kc in range(KC):
                    P.op("pe", lambda eng, pb=pb, kc=kc, n=n: eng.matmul(psum[pb][:], lhsT=mgT[:, kc, :], rhs=wout[:, kc, n * 512:(n + 1) * 512],
                                                                         start=(kc == 0), stop=(kc == KC - 1)),
                         reads=[HT, BW], writes=[pbuf[pb]], acc=(kc > 0))
                P.op("dve", lambda eng, pb=pb, n=n: eng.scalar_tensor_tensor(
                    out=htok[xb][:, n * 512:(n + 1) * 512], in0=htok[xb][:, n * 512:(n + 1) * 512], scalar=ALPHA, in1=psum[pb][:],
                    op0=ALU.mult, op1=ALU.add),
                    reads=[pbuf[pb]], writes=[H])
                yield
            h2 = htok[xb]
            layer_norm(h2, h2, ln1g, ln1b, H, H, [BW])
            dbg_store("h1", i, h2[:], H, D)
            if do_moe:
                P.dma("pool", h2_d[i * 128:(i + 1) * 128, :], h2[:], reads=[H], writes=[bufof("h2_d")], acc=True, stream="s1")
            P.op("act", lambda eng: eng.copy(out=h2bf[:], in_=h2[:]), reads=[H], writes=[HB])
            transpose_to(h2bf, KC, h2T[:].rearrange("p k t -> p (k t)"), HB, HT)
            yield
            if do_moe:
                P.dma("pool", h2T_d[:, :, i * 128:(i + 1) * 128], h2T[:], reads=[HT], writes=[bufof("h2T_d")], acc=True, stream="s2")
            pb = gen.get()
            for kc in range(KC):
                P.op("pe", lambda eng, pb=pb, kc=kc: eng.matmul(psum[pb][:, 0:36], lhsT=h2T[:, kc, :], rhs=wr[:, kc, :],
                                                                start=(kc == 0), stop=(kc == KC - 1)),
                     reads=[HT, BW], writes=[pbuf[pb]], acc=(kc > 0))
            RT, RS = bufof("rt"), bufof("rsm")
            P.op("dve", lambda eng, pb=pb: eng.tensor_tensor(out=rt[:], in0=psum[pb][:, 0:36], in1=brt[:], op=ALU.add),
                 reads=[pbuf[pb], BW], writes=[RT])
            P.op("dve", lambda eng: eng.tensor_reduce(out=rsm[:, 0:1], in_=rt[:, 0:4], axis=AX.X, op=ALU.max), reads=[RT], writes=[RS])
            P.op("dve", lambda eng: eng.tensor_scalar(out=rsm[:, 1:2], in0=rsm[:, 0:1], scalar1=-1.0, scalar2=None, op0=ALU.mult), writes=[RS])
            yield
            P.op("act", lambda eng: eng.activation(out=rsm[:, 12:16], in_=rt[:, 0:4], func=AF.Exp, bias=rsm[:, 1:2], scale=1.0,
                                                   accum_out=rsm[:, 2:3]), reads=[RT], writes=[RS])
            P.op("dve", lambda eng: eng.reciprocal(out=rsm[:, 3:4], in_=rsm[:, 2:3]), writes=[RS])
            P.op("dve", lambda eng: eng.tensor_scalar(out=rsm[:, 4:8], in0=rt[:, 0:4], scalar1=rsm[:, 0:1], scalar2=-1e30,
                                                      op0=ALU.is_lt, op1=ALU.mult), reads=[RT], writes=[RS])
            P.op("dve", lambda eng: eng.tensor_tensor(out=rt[:, 4:36].rearrange("p (g j) -> p g j", j=8),
                                                      in0=rt[:, 4:36].rearrange("p (g j) -> p g j", j=8),
                                                      in1=rsm[:, 4:8].unsqueeze(2).to_broadcast([128, 4, 8]), op=ALU.add),
                 reads=[RS], writes=[RT])
            P.op("dve", lambda eng: eng.max(out=m8[:], in_=rt[:, 4:36]), reads=[RT], writes=[bufof("m8")])
            P.op("dve", lambda eng: eng.tensor_tensor(out=rsm[:, 8:9], in0=m8[:, 1:2], in1=m8[:, 0:1], op=ALU.subtract),
                 reads=[bufof("m8")], writes=[RS])
            yield
            P.op("act", lambda eng: eng.activation(out=rsm[:, 9:10], in_=rsm[:, 8:9], func=AF.Exp), writes=[RS])
            P.op("dve", lambda eng: eng.tensor_scalar(out=rsm[:, 10:11], in0=rsm[:, 9:10], scalar1=1.0, scalar2=None, op0=ALU.add), writes=[RS])
            P.op("dve", lambda eng: eng.reciprocal(out=rsm[:, 10:11], in_=rsm[:, 10:11]), writes=[RS])
            P.op("dve", lambda eng: eng.scalar_tensor_tensor(out=rsm[:, 10:11], in0=rsm[:, 10:11], scalar=1.0 / ALPHA, in1=rsm[:, 3:4],
                                                             op0=ALU.mult, op1=ALU.mult), writes=[RS])
            P.op("dve", lambda eng: eng.tensor_tensor(out=rsm[:, 11:12], in0=rsm[:, 10:11], in1=rsm[:, 9:10], op=ALU.mult), writes=[RS])
            P.op("dve", lambda eng: eng.tensor_scalar(out=g1t[:], in0=rt[:, 4:36], scalar1=m8[:, 0:1], scalar2=rsm[:, 10:11],
                                                      op0=ALU.is_equal, op1=ALU.mult), reads=[RT, RS, bufof("m8")], writes=[bufof("g1t")])
            P.op("dve", lambda eng: eng.tensor_scalar(out=g2t[:], in0=rt[:, 4:36], scalar1=m8[:, 1:2], scalar2=rsm[:, 11:12],
                                                      op0=ALU.is_equal, op1=ALU.mult), reads=[RT, RS, bufof("m8")], writes=[bufof("g2t")])
            P.op("dve", lambda eng: eng.tensor_tensor(out=gtall[:, i, :], in0=g1t[:], in1=g2t[:], op=ALU.add),
                 reads=[bufof("g1t"), bufof("g2t")], writes=[bufof("gtall")], acc=True)
            dbg_store("gm", i, gtall[:, i, :], bufof("gtall"), 32)
            if not do_moe:
                final.append(P.dma("pool", out_d[i * 128:(i + 1) * 128, :], h2[:], reads=[H], stream="o"))

        for it in range(ntiles + 2):
            g3 = stage_S3(it - 2) if it - 2 >= 0 else None
            g2 = stage_S2(it - 1) if 0 <= it - 1 < ntiles else None
            if it < ntiles:
                stage_X(it, g3, g2)
                rem = it - it // 3
                n2 = max(1, (rem + NBIS - 3) // (NBIS - 2))
                stage_Y(it, g3, g2, n2)
                stage_W(it)
            else:
                drain(g3)
                drain(g2)

        if do_moe:
            last = []
            for e, L in P.L.items():
                for it in reversed(L):
                    if isinstance(it, tuple):
                        last.append(it[1])
                        break
            BAR = Buf()
            for t in last:
                BAR.w[id(t.sem)] = t
            for nm in ("h2_d", "h2T_d"):
                for k, t in bufof(nm).w.items():
                    BAR.w[k] = t
            TB = 1024
            ntok = ntiles * 128
            nblk = (ntok + TB - 1) // TB
            o = 0
            wgb, wub, wdb, stgb = [], [], [], []
            for par in range(2):
                wgb.append(arena[:, o:o + 2048].rearrange("p (k f) -> p k f", k=KC)); o += 2048
                wub.append(arena[:, o:o + 2048].rearrange("p (k f) -> p k f", k=KC)); o += 2048
                wdb.append(arena[:, o:o + 2048].rearrange("p (k f) -> p k f", k=2)); o += 2048
            for j in range(3):
                stgb.append(arena[:, o:o + 4096].bitcast(F32)); o += 4096
            accv = arena[:, o:o + 16384].bitcast(F32).rearrange("p (a d) -> p a d", a=8); o += 16384
            hblk = arena[:, o:o + 8192].rearrange("p (k t) -> p k t", k=KC); o += 8192
            sgt, hid = [], []
            for j in range(2):
                sgt.append(arena[:, o:o + 512]); o += 512
            for j in range(3):
                hid.append(arena[:, o:o + 1024].rearrange("p (f t) -> p f t", f=2)); o += 1024
            assert o <= A_END, (o, A_END)
            P.dma("sp", lng[:], ln2g_d, reads=[BAR], writes=[bufof("lng")], stream="lg")
            P.dma("sp", lnb[:], ln2b_d, reads=[BAR], writes=[bufof("lnb")], stream="lb")

            items = []
            for blk in range(nblk):
                t0 = blk * TB
                tb = min(TB, ntok - t0)
                for e in range(nexp):
                    for sub in range(0, tb, 512):
                        items.append((blk, e, t0, sub, min(512, tb - sub)))
            eseq = []
            for it in items:
                if not eseq or eseq[-1] != (it[0], it[1]):
                    eseq.append((it[0], it[1]))
            item_j = []
            jj = -1
            prev = None
            for it in items:
                if (it[0], it[1]) != prev:
                    jj += 1
                    prev = (it[0], it[1])
                item_j.append(jj)
            srcs = (wg_d, wu_d, wd_d)

            def emit_stage(j):
                if j >= len(eseq):
                    return
                e = eseq[j][1]
                for m in range(3):
                    kk = 8 if m < 2 else 2
                    P.dma("sp", stgb[m][:].rearrange("p (k f) -> p k f", k=kk), srcs[m][e], reads=[BAR],
                          writes=[bufof("stgb%d" % m)], stream="ws%d" % m)

            def emit_cast(j):
                if j >= len(eseq):
                    return
                par = j % 2
                dsts = (wgb[par], wub[par], wdb[par])
                for m in range(3):
                    WB = bufof("wb%d_%d" % (m, par))
                    if m < 2:
                        P.op("act", lambda eng, d=dsts[m], m=m: eng.copy(out=d.rearrange("p k f -> p (k f)"), in_=stgb[m][:]),
                             reads=[bufof("stgb%d" % m), BAR], writes=[WB])
                    else:
                        P.op("dve", lambda eng, d=dsts[m], m=m: eng.tensor_copy(out=d.rearrange("p k f -> p (k f)"), in_=stgb[m][:]),
                             reads=[bufof("stgb%d" % m), BAR], writes=[WB])

            gu_banks = ((0, 1), (2, 3))
            gu_n = [0]
            d_n = [0]
            rl_n = [0]
            loaded_blk = [-1]

            def emit_GU(s, f):
                if s >= len(items):
                    return
                blk, e, t0, sub, sw = items[s]
                par = item_j[s] % 2
                hh_ = sub // 512
                HBK = bufof("hblk%d" % hh_)
                if f == 0 and e == 0 and blk == 0:
                    P.dma("sp", hblk[:, :, sub:sub + sw], h2T_d[:, :, t0 + sub:t0 + sub + sw], reads=[BAR], writes=[HBK], stream="hb%d" % hh_)
                pg, pu = gu_banks[gu_n[0] % 2]
                sp_ = gu_n[0] % 2
                gu_n[0] += 1
                hb_ = s % 3
                WG, WU = bufof("wb0_%d" % par), bufof("wb1_%d" % par)
                for kc in range(KC):
                    P.op("pe", lambda eng, kc=kc: eng.matmul(psum[pg][:, 0:sw], lhsT=wgb[par][:, kc, f * 128:(f + 1) * 128],
                                                             rhs=hblk[:, kc, sub:sub + sw], start=(kc == 0), stop=(kc == KC - 1)),
                         reads=[WG, HBK, BAR], writes=[pbuf[pg]], acc=(kc > 0))
                for kc in range(KC):
                    P.op("pe", lambda eng, kc=kc: eng.matmul(psum[pu][:, 0:sw], lhsT=wub[par][:, kc, f * 128:(f + 1) * 128],
                                                             rhs=hblk[:, kc, sub:sub + sw], start=(kc == 0), stop=(kc == KC - 1)),
                         reads=[WU, HBK, BAR], writes=[pbuf[pu]], acc=(kc > 0))
                SG = bufof("sgt%d" % sp_)
                P.op("act", lambda eng: eng.activation(out=sgt[sp_][:, 0:sw], in_=psum[pg][:, 0:sw], func=AF.Silu),
                     reads=[pbuf[pg], BAR], writes=[SG])
                P.op("dve", lambda eng: eng.tensor_tensor(out=hid[hb_][:, f, 0:sw], in0=sgt[sp_][:, 0:sw], in1=psum[pu][:, 0:sw], op=ALU.mult),
                     reads=[SG, pbuf[pu], BAR], writes=[bufof("hid%d_%d" % (hb_, f))])
                if f == 1 and e == nexp - 1 and blk + 1 < nblk:
                    t1 = (blk + 1) * TB
                    sw1 = min(512, ntok - (t1 + sub))
                    if sw1 > 0:
                        P.dma("sp", hblk[:, :, sub:sub + sw1], h2T_d[:, :, t1 + sub:t1 + sub + sw1], reads=[BAR], writes=[HBK], stream="hb%d" % hh_)

            def emit_D(s):
                blk, e, t0, sub, sw = items[s]
                par = item_j[s] % 2
                hb_ = s % 3
                WD = bufof("wb2_%d" % par)
                g = 0
                for tt in range(sw // 128):
                    ta = (sub // 128) + tt
                    tg = t0 // 128 + ta
                    for n in range(2):
                        pd = 4 + d_n[0] % 4
                        d_n[0] += 1
                        for f in range(2):
                            P.op("pe", lambda eng, f=f, tt=tt, n=n, pd=pd: eng.matmul(
                                psum[pd][:], lhsT=hid[hb_][:, f, tt * 128:(tt + 1) * 128], rhs=wdb[par][:, f, n * 512:(n + 1) * 512],
                                start=(f == 0), stop=(f == 1)),
                                reads=[bufof("hid%d_%d" % (hb_, f)), WD], writes=[pbuf[pd]], acc=(f > 0))
                        ACC = bufof("acc%d_%d" % (ta, n))
                        av = accv[:, ta, n * 512:(n + 1) * 512]
                        gsc = gtall[:, tg, e:e + 1]
                        if g not in (1, 4, 7):
                            P.op("dve", lambda eng, pd=pd, av=av, gsc=gsc: eng.scalar_tensor_tensor(out=av, in0=psum[pd][:], scalar=gsc, in1=av,
                                                                                                   op0=ALU.mult, op1=ALU.add),
                                 reads=[pbuf[pd], bufof("gtall"), BAR], writes=[ACC])
                        else:
                            r = rl_n[0] % NRL
                            rl_n[0] += 1
                            RL = bufof("rl%d" % r)
                            P.op("act", lambda eng, pd=pd, r=r, gsc=gsc: eng.activation(out=rl[r][:], in_=psum[pd][:], func=AF.Copy, scale=gsc),
                                 reads=[pbuf[pd], bufof("gtall"), BAR], writes=[RL])
                            P.op("pool", lambda eng, r=r, av=av: eng.tensor_add(out=av, in0=av, in1=rl[r][:]),
                                 reads=[RL, BAR], writes=[ACC])
                        g += 1

            def emit_acc_init(blk, ta):
                t0 = blk * TB
                if t0 + ta * 128 >= ntok:
                    return
                tg = t0 // 128 + ta
                ACC = [bufof("acc%d_0" % ta), bufof("acc%d_1" % ta)]
                P.dma("sp", accv[:, ta, :], h2_d[tg * 128:(tg + 1) * 128, :], reads=[BAR], writes=ACC, stream="ai%d" % ta)

            def emit_block_final(blk, half):
                t0 = blk * TB
                tb = min(TB, ntok - t0)
                ta0 = half * 4
                ta1 = min(tb // 128, ta0 + 4)
                ntt = ta1 - ta0
                if ntt <= 0:
                    return
                MV8, RS8 = bufof("mv8_%d" % half), bufof("rs8_%d" % half)
                for ta in range(ta0, ta1):
                    ACC0, ACC1 = bufof("acc%d_0" % ta), bufof("acc%d_1" % ta)
                    tg = t0 // 128 + ta
                    if "ffn" in dbg_d:
                        final.append(P.dma("pool", dbg_d["ffn"][tg * 128:(tg + 1) * 128, :], accv[:, ta, :], reads=[ACC0, ACC1], stream="o"))
                    src = accv[:, ta, :]
                    for c in range(2):
                        P.op("dve", lambda eng, c=c, src=src: eng.bn_stats(out=stats[:, c, :], in_=src[:, c * 512:(c + 1) * 512]),
                             reads=[ACC0 if c == 0 else ACC1], writes=[bufof("stats")], acc=(c > 0))
                    P.op("dve", lambda eng, ta=ta: eng.bn_aggr(out=mv8[:, ta, :], in_=stats[:].rearrange("p a s -> p (a s)")),
                         reads=[bufof("stats")], writes=[MV8], acc=(ta > ta0))
                P.op("act", lambda eng: eng.activation(out=rs8[:, ta0:ta1], in_=mv8[:, ta0:ta1, 1], func=AF.Sqrt, bias=eps2t[:], scale=1.0),
                     reads=[MV8, BW], writes=[RS8])
                P.op("dve", lambda eng: eng.reciprocal(out=rs8[:, ta0:ta1], in_=rs8[:, ta0:ta1]), writes=[RS8])
                P.op("dve", lambda eng: eng.scalar_tensor_tensor(out=rs8[:, 8 + ta0:8 + ta1], in0=mv8[:, ta0:ta1, 0], scalar=-1.0, in1=rs8[:, ta0:ta1],
                                                                 op0=ALU.mult, op1=ALU.mult),
                     reads=[MV8], writes=[RS8])
                for ta in range(ta0, ta1):
                    ACC0, ACC1 = bufof("acc%d_0" % ta), bufof("acc%d_1" % ta)
                    tg = t0 // 128 + ta
                    xb = ta % 2
                    HH = bufof("htok%d" % xb)
                    src = accv[:, ta, :]
                    P.op("act", lambda eng, xb=xb, src=src, ta=ta: eng.activation(out=htok[xb][:], in_=src, func=AF.Identity,
                                                                                 bias=rs8[:, 8 + ta:9 + ta], scale=rs8[:, ta:ta + 1]),
                         reads=[ACC0, ACC1, RS8], writes=[HH])
                    if blk + 1 < nblk:
                        emit_acc_init(blk + 1, ta)
                    P.op("dve", lambda eng, xb=xb: eng.tensor_mul(out=htok[xb][:], in0=htok[xb][:], in1=lng[:]),
                         reads=[bufof("lng")], writes=[HH])
                    P.op("dve", lambda eng, xb=xb: eng.tensor_add(out=htok[xb][:], in0=htok[xb][:], in1=lnb[:]),
                         reads=[bufof("lnb")], writes=[HH])
                    final.append(P.dma("pool", out_d[tg * 128:(tg + 1) * 128, :], htok[xb][:], reads=[HH], stream="o%d" % xb))

            for ta in range(TB // 128):
                emit_acc_init(0, ta)
            emit_stage(0)
            emit_cast(0)
            emit_stage(1)
            emit_cast(1)
            emit_stage(2)
            emit_GU(0, 0)
            emit_GU(0, 1)
            emit_GU(1, 0)
            N = len(items)
            for s in range(N):
                emit_D(s)
                last_of_expert = (s == N - 1) or (item_j[s + 1] != item_j[s])
                if last_of_expert:
                    j = item_j[s]
                    emit_cast(j + 2)
                    emit_stage(j + 3)
                if items[s][1] == nexp - 1:
                    emit_block_final(items[s][0], items[s][3] // 512)
                emit_GU(s + 1, 1)
                emit_GU(s + 2, 0)

        fw = {}
        for t in final:
            k = id(t.sem)
            if k not in fw or fw[k].val < t.val:
                fw[k] = t
        P.finalize(list(fw.values()))
        print("ops traced:", P.nops, flush=True)
    return nc


def host_inputs(inputs):
    f = np.float32
    bf = ml_dtypes.bfloat16
    w_in = np.asarray(inputs["w_in"][0], dtype=f)
    o = np.cumsum([0, 512, 512, 64, 64, 256, 32, 8, 2048])
    c_up, c_q, c_k, c_v, c_iq, c_ik, c_iw, c_g = [slice(o[j], o[j + 1]) for j in range(8)]
    w_tok = np.ascontiguousarray(np.concatenate([w_in[:, c_up], w_in[:, c_g], w_in[:, c_v], w_in[:, c_iw]], axis=1))
    w_feat = np.ascontiguousarray(np.concatenate([w_in[:, c_q], w_in[:, c_k], w_in[:, c_iq]] + [w_in[:, c_ik]] * 3, axis=1))

    def rep(v):
        v = np.asarray(v, dtype=f).reshape(1, -1)
        return np.ascontiguousarray(np.broadcast_to(v, (128, v.shape[1])))

    def col(v):
        return np.ascontiguousarray(np.asarray(v, dtype=f).reshape(KC, 128).T)

    band = np.zeros((128, 12, 128), dtype=f)
    tp = np.arange(128)[:, None]
    tq = np.arange(128)[None, :]
    for g, w in enumerate((2, 4, 8, 16)):
        cur = ((tp <= tq) & (tp > tq - w)).astype(f) / w - (tp == tq).astype(f)
        prev = ((tp - 128) > (tq - w)).astype(f) / w
        cnt = np.minimum(w, tq + 1).astype(f)
        cur0 = ((tp <= tq) & (tp > tq - w)).astype(f) / cnt - (tp == tq).astype(f)
        band[:, g], band[:, 4 + g], band[:, 8 + g] = cur, prev, cur0
    cbias = np.where(tq <= tp, 0.0, -1e30).astype(f)
    pow2 = np.broadcast_to((2.0 ** -np.arange(NBIS + 1)).astype(f)[None, :], (128, NBIS + 1))
    common = {
        "w_in_tok": w_tok, "w_in_feat": w_feat,
        "ln_in_g": rep(inputs["ln_in_g"]), "ln_in_b": rep(inputs["ln_in_b"]),
        "ln1_g": rep(inputs["ln1_g"][0]), "ln1_b": rep(inputs["ln1_b"][0]),
        "ln2_g": rep(inputs["ln2_g"][0]), "ln2_b": rep(inputs["ln2_b"][0]),
        "ln_in_gc": col(inputs["ln_in_g"]), "ln_in_bc": col(inputs["ln_in_b"]),
        "ln1_gc": col(inputs["ln1_g"][0]), "ln1_bc": col(inputs["ln1_b"][0]),
        "ident": np.eye(128, dtype=f).astype(bf),
        "i4": np.ascontiguousarray(np.tile(np.eye(128, dtype=f), (1, 4))).astype(bf),
        "band": np.ascontiguousarray(band.reshape(128, 12 * 128)).astype(bf),
        "cbias": np.ascontiguousarray(cbias),
        "pow2": np.ascontiguousarray(pow2),
        "b_gate": np.ascontiguousarray(np.asarray(inputs["b_gate"], dtype=f).reshape(1, 2048)),
        "pool_w": np.ascontiguousarray(np.asarray(inputs["pool_w"][0], dtype=f)),
        "pool_scale": np.ascontiguousarray(np.asarray(inputs["pool_scale"][0], dtype=f).reshape(4, 128).T),
        "w_proj_pool": np.ascontiguousarray(np.asarray(inputs["w_proj_pool"][0], dtype=f)),
        "w_proj_attn": np.ascontiguousarray(np.asarray(inputs["w_proj_attn"][0], dtype=f)),
        "w_out": np.ascontiguousarray(np.asarray(inputs["w_out"][0], dtype=f)),
        "w_gr": np.ascontiguousarray(np.concatenate([np.asarray(inputs["w_group"][0], dtype=f),
                                                     np.asarray(inputs["w_router"][0], dtype=f)], axis=1)),
        "b_gr": rep(np.concatenate([np.asarray(inputs["b_group"][0], dtype=f), np.asarray(inputs["b_router"][0], dtype=f)])),
        "w_gate": np.ascontiguousarray(np.asarray(inputs["w_gate"][0], dtype=f)),
        "w_up": np.ascontiguousarray(np.asarray(inputs["w_up"][0], dtype=f)),
        "w_down": np.ascontiguousarray(np.asarray(inputs["w_down"][0], dtype=f)),
    }
    maps = []
    for c in range(8):
        m = dict(common)
        m["x"] = np.ascontiguousarray(np.asarray(inputs["x"][c], dtype=f))
        maps.append(m)
    return maps


def kernel(**inputs):
    nc = build({})
    maps = host_inputs(inputs)
    res = run_bass_kernel_spmd(nc, maps, core_ids=list(range(8)))
    return np.stack([r["out"] for r in res.results], axis=0).astype(np.float32)
```
